# Optimizing a Trainium2 kernel written in Bass

```python
import math
import jax, jax.numpy as jnp
from jax import lax
import numpy as np

D_MODEL = 4096
BATCH = 1
SEQ = 8192
DEPTH = 4

CTX_LEN = 256
GRID_W = 64
HEAD_DIM = 128
ROPE_THETA = 10000.0
Q_BLOCK = 128
LN_EPS = 1e-5
RMS_EPS = 1e-6

DIFF_HEADS = (D_MODEL // 2) // (2 * HEAD_DIM)
DIFF_V_DIM = 2 * HEAD_DIM
DIFF_QK = DIFF_HEADS * 2 * HEAD_DIM
DIFF_V = DIFF_HEADS * DIFF_V_DIM
GQA_HEADS = (D_MODEL // 2) // HEAD_DIM
GQA_KV_HEADS = GQA_HEADS // 4
GQA_Q = GQA_HEADS * HEAD_DIM
GQA_KV = GQA_KV_HEADS * HEAD_DIM
EVEN_SPLITS = [DIFF_QK, 2 * DIFF_QK, 2 * DIFF_QK + DIFF_V,
               2 * DIFF_QK + DIFF_V + GQA_Q, 2 * DIFF_QK + DIFF_V + GQA_Q + GQA_KV]
EVEN_IN = 2 * DIFF_QK + DIFF_V + GQA_Q + 2 * GQA_KV
EVEN_OUT = DIFF_V + GQA_Q

MLA_HEADS = D_MODEL // HEAD_DIM
MLA_Q_RANK = D_MODEL // 4
MLA_KV_RANK = D_MODEL // 8
MLA_NOPE = 128
MLA_ROPE = 64
MLA_V = 128
ODD_IN = MLA_Q_RANK + MLA_KV_RANK + MLA_ROPE
ODD_OUT = MLA_HEADS * MLA_V

N_GROUPS = 4
EXPERTS_PER_GROUP = 4
N_EXPERTS = N_GROUPS * EXPERTS_PER_GROUP
EXPERT_TOP_K = 2
D_EXPERT = 384

N_MOD = 6
DEEPNORM_ALPHA = (2.0 * DEPTH) ** 0.25
DEEPNORM_BETA = (8.0 * DEPTH) ** -0.25
N_EVEN = (DEPTH + 1) // 2
N_ODD = DEPTH // 2

kernel_name = "hybrid_diffgqa_mla_hiermoe_dit"


def _layer_norm(x, g, b):
    xf = x.astype(jnp.float32)
    mu = jnp.mean(xf, axis=-1, keepdims=True)
    var = jnp.mean(jnp.square(xf - mu), axis=-1, keepdims=True)
    y = (xf - mu) * lax.rsqrt(var + LN_EPS)
    return (y * g + b).astype(x.dtype)


def _rms_norm(x, g):
    xf = x.astype(jnp.float32)
    y = xf * lax.rsqrt(jnp.mean(jnp.square(xf), axis=-1, keepdims=True) + RMS_EPS)
    return (y * g).astype(x.dtype)


def _axial_rope_tables(n_tokens, rot_dim):
    rows = n_tokens // GRID_W
    row, col = jnp.meshgrid(jnp.arange(rows), jnp.arange(GRID_W), indexing="ij")
    row = row.reshape(-1).astype(jnp.float32)
    col = col.reshape(-1).astype(jnp.float32)
    quarter = rot_dim // 4
    inv_freq = ROPE_THETA ** (-jnp.arange(quarter, dtype=jnp.float32) / quarter)
    ang = jnp.concatenate([row[:, None] * inv_freq, col[:, None] * inv_freq], axis=-1)
    return jnp.cos(ang), jnp.sin(ang)


def _apply_rope(x, cos, sin):
    half = x.shape[-1] // 2
    xf = x.astype(jnp.float32)
    x1, x2 = xf[..., :half], xf[..., half:]
    c, s = cos[None, :, None, :], sin[None, :, None, :]
    return jnp.concatenate([x1 * c - x2 * s, x1 * s + x2 * c], axis=-1).astype(x.dtype)


def _block_attention(q, k, v, scale):
    B, S, H, d = q.shape
    G = k.shape[2]
    rep = H // G
    nb = S // Q_BLOCK
    qb = q.reshape(B, nb, Q_BLOCK, G, rep, d).transpose(1, 0, 2, 3, 4, 5)

    def one_block(q_blk):
        s = jnp.einsum("bqgrd,bkgd->bgrqk", q_blk, k, preferred_element_type=jnp.float32) * scale
        p = jax.nn.softmax(s, axis=-1).astype(v.dtype)
        return jnp.einsum("bgrqk,bkgv->bqgrv", p, v)

    o = lax.map(one_block, qb)
    return o.transpose(1, 0, 2, 3, 4, 5).reshape(B, S, H, v.shape[-1])


def _diff_lambda(lam_vecs, lambda_init):
    lv = lam_vecs.astype(jnp.float32)
    return jnp.exp(jnp.sum(lv[0] * lv[1])) - jnp.exp(jnp.sum(lv[2] * lv[3])) + lambda_init


def _diff_attention(q, k, v, lam, subln_g, lambda_init):
    B, S = q.shape[:2]
    scale = 1.0 / math.sqrt(HEAD_DIM)
    a1 = _block_attention(q[:, :, 0::2], k[:, :, 0::2], v, scale)
    a2 = _block_attention(q[:, :, 1::2], k[:, :, 1::2], v, scale)
    o = a1 - lam.astype(a1.dtype) * a2
    o = _rms_norm(o, subln_g) * (1.0 - lambda_init)
    return o.reshape(B, S, DIFF_V)


def _gqa(q, k, v):
    B, S = q.shape[:2]
    return _block_attention(q, k, v, 1.0 / math.sqrt(HEAD_DIM)).reshape(B, S, GQA_Q)


def _split_even(p):
    B, T = p.shape[:2]
    qa, ka, va, qg, kg, vg = jnp.split(p, EVEN_SPLITS, axis=-1)
    return (qa.reshape(B, T, 2 * DIFF_HEADS, HEAD_DIM),
            ka.reshape(B, T, 2 * DIFF_HEADS, HEAD_DIM),
            va.reshape(B, T, DIFF_HEADS, DIFF_V_DIM),
            qg.reshape(B, T, GQA_HEADS, HEAD_DIM),
            kg.reshape(B, T, GQA_KV_HEADS, HEAD_DIM),
            vg.reshape(B, T, GQA_KV_HEADS, HEAD_DIM))


def _even_mixer(h_l, h_c, w_in, w_out, lam_vecs, subln_g, qn_g, kn_g, lambda_init, cos, sin, need_ctx):
    qa_l, ka_l, va_l, qg_l, kg_l, vg_l = _split_even(h_l @ w_in)
    qa_c, ka_c, va_c, qg_c, kg_c, vg_c = _split_even(h_c @ w_in)
    lam = _diff_lambda(lam_vecs, lambda_init)
    qa_l = _apply_rope(qa_l, cos, sin)
    ka_l = _apply_rope(ka_l, cos, sin)
    qg_l = _apply_rope(_rms_norm(qg_l, qn_g), cos, sin)
    kg_l = _apply_rope(_rms_norm(kg_l, kn_g), cos, sin)
    kg_c = _rms_norm(kg_c, kn_g)
    ka_all = jnp.concatenate([ka_l, ka_c], axis=1)
    va_all = jnp.concatenate([va_l, va_c], axis=1)
    kg_all = jnp.concatenate([kg_l, kg_c], axis=1)
    vg_all = jnp.concatenate([vg_l, vg_c], axis=1)
    o_l = jnp.concatenate([_diff_attention(qa_l, ka_all, va_all, lam, subln_g, lambda_init),
                           _gqa(qg_l, kg_all, vg_all)], axis=-1) @ w_out
    o_c = None
    if need_ctx:
        o_c = jnp.concatenate([_diff_attention(qa_c, ka_c, va_c, lam, subln_g, lambda_init),
                               _gqa(_rms_norm(qg_c, qn_g), kg_c, vg_c)], axis=-1) @ w_out
    return o_l, o_c


def _mla_project(h, w_in, qn_g, kvn_g, w_uq, w_ukv):
    B, T = h.shape[:2]
    c_q, c_kv, k_rope = jnp.split(h @ w_in, [MLA_Q_RANK, MLA_Q_RANK + MLA_KV_RANK], axis=-1)
    q = (_rms_norm(c_q, qn_g) @ w_uq).reshape(B, T, MLA_HEADS, MLA_NOPE + MLA_ROPE)
    kv = (_rms_norm(c_kv, kvn_g) @ w_ukv).reshape(B, T, MLA_HEADS, MLA_NOPE + MLA_V)
    return (q[..., :MLA_NOPE], q[..., MLA_NOPE:], kv[..., :MLA_NOPE], kv[..., MLA_NOPE:],
            k_rope.reshape(B, T, 1, MLA_ROPE))


def _mla_keys(k_nope, k_rope):
    return jnp.concatenate([k_nope, jnp.broadcast_to(k_rope, k_nope.shape[:-1] + (MLA_ROPE,))], axis=-1)


def _odd_mixer(h_l, h_c, w_in, qn_g, kvn_g, w_uq, w_ukv, w_out, cos, sin, need_ctx):
    B, S = h_l.shape[:2]
    scale = 1.0 / math.sqrt(MLA_NOPE + MLA_ROPE)
    qn_l, qr_l, kn_l, v_l, kr_l = _mla_project(h_l, w_in, qn_g, kvn_g, w_uq, w_ukv)
    qn_c, qr_c, kn_c, v_c, kr_c = _mla_project(h_c, w_in, qn_g, kvn_g, w_uq, w_ukv)
    q_l = jnp.concatenate([qn_l, _apply_rope(qr_l, cos, sin)], axis=-1)
    k_l = _mla_keys(kn_l, _apply_rope(kr_l, cos, sin))
    k_c = _mla_keys(kn_c, kr_c)
    o_l = _block_attention(q_l, jnp.concatenate([k_l, k_c], axis=1),
                           jnp.concatenate([v_l, v_c], axis=1), scale).reshape(B, S, ODD_OUT) @ w_out
    o_c = None
    if need_ctx:
        q_c = jnp.concatenate([qn_c, qr_c], axis=-1)
        o_c = _block_attention(q_c, k_c, v_c, scale).reshape(B, h_c.shape[1], ODD_OUT) @ w_out
    return o_l, o_c


def _hier_moe(x, w_group, b_group, w_router, b_router, w_gate, w_up, w_down):
    B, T, D = x.shape
    xf = x.reshape(B * T, D)
    group_prob = jax.nn.softmax((xf @ w_group + b_group).astype(jnp.float32), axis=-1)
    group_idx = jnp.argmax(group_prob, axis=-1)
    group_w = jnp.take_along_axis(group_prob, group_idx[:, None], axis=-1)
    e_logits = (xf @ w_router + b_router).astype(jnp.float32).reshape(-1, N_GROUPS, EXPERTS_PER_GROUP)
    in_group = jnp.take_along_axis(e_logits, group_idx[:, None, None], axis=1)[:, 0]
    top_p, top_i = lax.top_k(jax.nn.softmax(in_group, axis=-1), EXPERT_TOP_K)
    top_p = top_p / jnp.sum(top_p, axis=-1, keepdims=True)
    expert_id = group_idx[:, None] * EXPERTS_PER_GROUP + top_i
    gates = jnp.einsum("nk,nke->ne", group_w * top_p,
                       jax.nn.one_hot(expert_id, N_EXPERTS, dtype=jnp.float32))
    hidden = jax.nn.silu(jnp.einsum("nd,edf->nef", xf, w_gate)) * jnp.einsum("nd,edf->nef", xf, w_up)
    y = jnp.einsum("nef,efd->nd", hidden * gates[:, :, None].astype(hidden.dtype), w_down)
    return y.reshape(B, T, D)


def setup_inputs(seed: int = 0) -> dict:
    key = jax.random.key(seed)
    ks = jax.random.split(key, 32)

    def nrm(k, shape, scale):
        return jax.random.normal(k, shape, jnp.float32) * scale

    D = D_MODEL
    return {
        "x": nrm(ks[0], (BATCH, SEQ, D), 1.0),
        "c": nrm(ks[1], (BATCH, D), 1.0),
        "ctx": nrm(ks[2], (BATCH, CTX_LEN, D), 1.0),
        "c_ctx": nrm(ks[3], (D,), 1.0),
        "w_ada": nrm(ks[4], (DEPTH, D, N_MOD * D), 0.5 * D ** -0.5),
        "b_ada": nrm(ks[5], (DEPTH, N_MOD * D), 0.01),
        "ln1_g": 1.0 + nrm(ks[6], (DEPTH, D), 0.02),
        "ln1_b": nrm(ks[7], (DEPTH, D), 0.02),
        "ln2_g": 1.0 + nrm(ks[8], (DEPTH, D), 0.02),
        "ln2_b": nrm(ks[9], (DEPTH, D), 0.02),
        "ev_w_in": nrm(ks[10], (N_EVEN, D, EVEN_IN), D ** -0.5),
        "ev_w_out": nrm(ks[11], (N_EVEN, EVEN_OUT, D), EVEN_OUT ** -0.5 * DEEPNORM_BETA),
        "diff_lambda": nrm(ks[12], (N_EVEN, 4, HEAD_DIM), 0.1),
        "diff_subln_g": 1.0 + nrm(ks[13], (N_EVEN, DIFF_V_DIM), 0.02),
        "gqa_q_norm_g": 1.0 + nrm(ks[14], (N_EVEN, HEAD_DIM), 0.02),
        "gqa_k_norm_g": 1.0 + nrm(ks[15], (N_EVEN, HEAD_DIM), 0.02),
        "od_w_in": nrm(ks[16], (N_ODD, D, ODD_IN), D ** -0.5),
        "mla_q_norm_g": 1.0 + nrm(ks[17], (N_ODD, MLA_Q_RANK), 0.02),
        "mla_kv_norm_g": 1.0 + nrm(ks[18], (N_ODD, MLA_KV_RANK), 0.02),
        "mla_w_uq": nrm(ks[19], (N_ODD, MLA_Q_RANK, MLA_HEADS * (MLA_NOPE + MLA_ROPE)), MLA_Q_RANK ** -0.5),
        "mla_w_ukv": nrm(ks[20], (N_ODD, MLA_KV_RANK, MLA_HEADS * (MLA_NOPE + MLA_V)), MLA_KV_RANK ** -0.5),
        "od_w_out": nrm(ks[21], (N_ODD, ODD_OUT, D), ODD_OUT ** -0.5 * DEEPNORM_BETA),
        "moe_w_group": nrm(ks[22], (DEPTH, D, N_GROUPS), D ** -0.5),
        "moe_b_group": nrm(ks[23], (DEPTH, N_GROUPS), 0.01),
        "moe_w_router": nrm(ks[24], (DEPTH, D, N_EXPERTS), D ** -0.5),
        "moe_b_router": nrm(ks[25], (DEPTH, N_EXPERTS), 0.01),
        "moe_w_gate": nrm(ks[26], (DEPTH, N_EXPERTS, D, D_EXPERT), D ** -0.5),
        "moe_w_up": nrm(ks[27], (DEPTH, N_EXPERTS, D, D_EXPERT), D ** -0.5),
        "moe_w_down": nrm(ks[28], (DEPTH, N_EXPERTS, D_EXPERT, D), D_EXPERT ** -0.5 * DEEPNORM_BETA),
    }


def reference(x, c, ctx, c_ctx, w_ada, b_ada, ln1_g, ln1_b, ln2_g, ln2_b,
              ev_w_in, ev_w_out, diff_lambda, diff_subln_g, gqa_q_norm_g, gqa_k_norm_g,
              od_w_in, mla_q_norm_g, mla_kv_norm_g, mla_w_uq, mla_w_ukv, od_w_out,
              moe_w_group, moe_b_group, moe_w_router, moe_b_router, moe_w_gate, moe_w_up, moe_w_down):
    n_lat = x.shape[1]
    cos_a, sin_a = _axial_rope_tables(n_lat, HEAD_DIM)
    cos_m, sin_m = _axial_rope_tables(n_lat, MLA_ROPE)
    silu_c = jax.nn.silu(c)
    silu_cc = jax.nn.silu(c_ctx)
    xl, xc = x, ctx
    for layer in range(DEPTH):
        need_ctx = layer < DEPTH - 1
        mods_l = [m[:, None, :] for m in jnp.split(silu_c @ w_ada[layer] + b_ada[layer], N_MOD, axis=-1)]
        mods_c = jnp.split(silu_cc @ w_ada[layer] + b_ada[layer], N_MOD, axis=-1)
        sh1_l, sc1_l, g1_l, sh2_l, sc2_l, g2_l = mods_l
        sh1_c, sc1_c, g1_c, sh2_c, sc2_c, g2_c = mods_c
        h_l = xl * (1.0 + sc1_l) + sh1_l
        h_c = xc * (1.0 + sc1_c) + sh1_c
        i = layer // 2
        if layer % 2 == 0:
            lambda_init = 0.8 - 0.6 * math.exp(-0.3 * layer)
            o_l, o_c = _even_mixer(h_l, h_c, ev_w_in[i], ev_w_out[i], diff_lambda[i], diff_subln_g[i],
                                   gqa_q_norm_g[i], gqa_k_norm_g[i], lambda_init, cos_a, sin_a, need_ctx)
        else:
            o_l, o_c = _odd_mixer(h_l, h_c, od_w_in[i], mla_q_norm_g[i], mla_kv_norm_g[i],
                                  mla_w_uq[i], mla_w_ukv[i], od_w_out[i], cos_m, sin_m, need_ctx)
        xl = _layer_norm(DEEPNORM_ALPHA * xl + g1_l * o_l, ln1_g[layer], ln1_b[layer])
        h2_l = xl * (1.0 + sc2_l) + sh2_l
        moe_args = (moe_w_group[layer], moe_b_group[layer], moe_w_router[layer], moe_b_router[layer],
                    moe_w_gate[layer], moe_w_up[layer], moe_w_down[layer])
        if need_ctx:
            xc = _layer_norm(DEEPNORM_ALPHA * xc + g1_c * o_c, ln1_g[layer], ln1_b[layer])
            h2_c = xc * (1.0 + sc2_c) + sh2_c
            n_ctx = xc.shape[1]
            f_all = _hier_moe(jnp.concatenate([h2_c, h2_l], axis=1), *moe_args)
            f_c, f_l = f_all[:, :n_ctx], f_all[:, n_ctx:]
            xc = _layer_norm(DEEPNORM_ALPHA * xc + g2_c * f_c, ln2_g[layer], ln2_b[layer])
        else:
            f_l = _hier_moe(h2_l, *moe_args)
        xl = _layer_norm(DEEPNORM_ALPHA * xl + g2_l * f_l, ln2_g[layer], ln2_b[layer])
    return xl
```

```python
import math
import numpy as np
import ml_dtypes
import concourse.bass as bass
import concourse.mybir as mybir
from concourse.bass_utils import run_bass_kernel_spmd

F32 = mybir.dt.float32
BF16 = mybir.dt.bfloat16
AF = mybir.ActivationFunctionType
ALU = mybir.AluOpType

NCORES = 8
D = 4096
KC = 32
SEQ = 8192
CTX = 256
LAT = SEQ // NCORES
CT = CTX // NCORES
T = LAT + CT
TT = [(0, 512), (512, 512), (1024, 32)]
NKB = (SEQ + CTX) // 128
DEPTH = 4
ALPHA = (2.0 * DEPTH) ** 0.25
LN_EPS = 1e-5
RMS_EPS = 1e-6
GRID_W = 64


class Buf:
    __slots__ = ("w", "r", "dsem", "dcnt", "name")

    def __init__(self, name=""):
        self.w = None
        self.r = []
        self.dsem = None
        self.dcnt = 0
        self.name = name


class Tile:
    def __init__(self, handle, name):
        self.t = handle
        self.b = Buf(name)

    def __getitem__(self, idx):
        return self.t[idx]


class Half(Tile):
    def __init__(self, handle, off, name):
        Tile.__init__(self, handle, name)
        self.off = off

    def __getitem__(self, idx):
        ps, cs = idx
        a = 0 if cs.start is None else cs.start
        b = 512 if cs.stop is None else cs.stop
        return self.t[ps, self.off + a:self.off + b]


class Prog:
    ENG = ("pe", "act", "dve", "pool", "sp")
    COMPUTE = ("pe", "act", "dve", "pool")

    def __init__(self):
        self.nc = bass.Bass("TRN2", target_bir_lowering=False)
        self.q = {e: [] for e in self.ENG}
        self.sems = []
        self.sem = {}
        self.cnt = {}
        for e in self.COMPUTE:
            self.sem[e] = self._newsem("c_" + e)
            self.cnt[e] = 0
        self.seen = {e: {} for e in self.ENG}
        self.sb_off = 16640
        self.sb_max = 229000
        self.nid = 0
        self.dma_bufs = []
        self.psum = []
        self.pp = []
        for i in range(4):
            h = self.nc.alloc_psum_tensor("pp%d" % i, [128, 1024], F32)
            self.pp.append(h)
            self.psum.append(Half(h, 0, "ps%d" % (2 * i)))
            self.psum.append(Half(h, 512, "ps%d" % (2 * i + 1)))

    def _newsem(self, name):
        h = self.nc.alloc_semaphore(name)
        self.sems.append(h)
        return (len(self.sems) - 1, h)

    def dram(self, name, shape, dtype, kind):
        return self.nc.dram_tensor(name, list(shape), dtype, kind=kind)

    def sb(self, name, shape, dtype):
        esz = 4 if dtype == F32 else 2
        per = esz
        for s in shape[1:]:
            per *= s
        per = (per + 63) // 64 * 64
        off = self.sb_off
        assert off + per <= self.sb_max, ("SBUF overflow", name, off, per)
        self.nid += 1
        h = self.nc.alloc_sbuf_tensor_at("%s_%d" % (name, self.nid), list(shape), dtype, offset=off)
        self.sb_off = off + per
        return Tile(h, name)

    def mark(self):
        return self.sb_off

    def release(self, m):
        self.sb_off = m

    def _collect(self, eng, reads, writes):
        need = {}

        def add(tok):
            if tok is None:
                return
            key, h, v, src = tok
            if src == "pe" and eng == "pe":
                return
            if self.seen[eng].get(key, 0) >= v:
                return
            if key not in need or need[key][1] < v:
                need[key] = (h, v)

        for b in reads:
            add(b.w)
        for b in writes:
            add(b.w)
            for t in b.r:
                add(t)
        out = []
        for key, (h, v) in need.items():
            self.seen[eng][key] = v
            out.append((h, v))
        return out

    def _commit(self, tok, reads, writes):
        for b in reads:
            b.r.append(tok)
        for b in writes:
            b.w = tok
            b.r = []

    def op(self, eng, fn, reads=(), writes=()):
        reads = [x.b if isinstance(x, Tile) else x for x in reads]
        writes = [x.b if isinstance(x, Tile) else x for x in writes]
        waits = self._collect(eng, reads, writes)
        self.cnt[eng] += 1
        key, h = self.sem[eng]
        tok = (key, h, self.cnt[eng], eng)
        self.q[eng].append((waits, fn, h, 1))
        self._commit(tok, reads, writes)

    def dma(self, queue, out_ap, in_ap, slot, reads=(), writes=()):
        reads = [x.b if isinstance(x, Tile) else x for x in reads]
        writes = [x.b if isinstance(x, Tile) else x for x in writes]
        slot = slot.b if isinstance(slot, Tile) else slot
        if slot.dsem is None:
            slot.dsem = self._newsem("d_%s_%d" % (slot.name, len(self.sems)))
            self.dma_bufs.append(slot)
        waits = self._collect(queue, reads, writes)
        slot.dcnt += 1
        key, h = slot.dsem
        tok = (key, h, 16 * slot.dcnt, "dma")
        self.q[queue].append((waits, lambda e: e.dma_start(out=out_ap, in_=in_ap), h, 16))
        self._commit(tok, reads, writes)

    def fence(self):
        cur = []
        for e in self.COMPUTE:
            key, h = self.sem[e]
            if self.cnt[e] > 0:
                cur.append((key, h, self.cnt[e]))
        for b in self.dma_bufs:
            key, h = b.dsem
            cur.append((key, h, 16 * b.dcnt))
        for e in self.ENG:
            waits = []
            for key, h, v in cur:
                if self.seen[e].get(key, 0) < v:
                    self.seen[e][key] = v
                    waits.append((h, v))
            if waits:
                self.q[e].append((waits, None, None, 0))

    def finish(self):
        self.fence()
        nc = self.nc
        q = self.q

        def replay(e, items):
            for waits, fn, h, inc in items:
                for (sh, v) in waits:
                    e.wait_ge(sh, v)
                if fn is not None:
                    ins = fn(e)
                    ins.then_inc(h, inc)

        with nc.Block() as block:
            @block.tensor
            def _(e):
                replay(e, q["pe"])

            @block.scalar
            def _(e):
                replay(e, q["act"])

            @block.vector
            def _(e):
                replay(e, q["dve"])

            @block.gpsimd
            def _(e):
                replay(e, q["pool"])

            @block.sync
            def _(e):
                replay(e, q["sp"])
        return nc


def run(prog_nc, in_maps):
    res = run_bass_kernel_spmd(prog_nc, in_maps, core_ids=list(range(NCORES)))
    return res.results


MCOLS = 6 * D // NCORES


def build_mods():
    P = Prog()
    nc = P.nc
    cT = P.dram("cT", [128, KC, 2], F32, "ExternalInput")
    wad = P.dram("wad", [DEPTH, D, MCOLS], F32, "ExternalInput")
    bad = P.dram("bad", [2, DEPTH * MCOLS], F32, "ExternalInput")
    mo = P.dram("mo", [2, DEPTH * MCOLS], F32, "ExternalOutput")
    c_sb = P.sb("c_sb", [128, KC, 2], F32)
    s_sb = P.sb("s_sb", [128, KC, 2], F32)
    b_sb = P.sb("b_sb", [2, DEPTH * MCOLS], F32)
    o_sb = P.sb("o_sb", [2, DEPTH * MCOLS], F32)
    MB = 256
    wts = [P.sb("wt%d" % i, [128, KC, MB], F32) for i in range(2)]
    P.dma("sp", c_sb[:], cT.ap(), c_sb, writes=[c_sb])
    P.dma("sp", b_sb[:], bad.ap(), b_sb, writes=[b_sb])
    P.op("act", lambda e: e.activation(out=s_sb[:], in_=c_sb[:], func=AF.Silu),
         reads=[c_sb], writes=[s_sb])
    nblk = DEPTH * MCOLS // MB
    for blk in range(nblk):
        l, cb = divmod(blk, MCOLS // MB)
        wt = wts[blk % 2]
        src = wad.ap()[l, :, cb * MB:(cb + 1) * MB].rearrange("(kc p) n -> p kc n", p=128)
        P.dma("sp", wt[:], src, wt, writes=[wt])
        ps = P.psum[blk % 2]

        def mm(e, wt=wt, ps=ps):
            ins = None
            for kc in range(KC):
                ins = e.matmul(ps[0:2, 0:MB], lhsT=s_sb[:, kc, :], rhs=wt[:, kc, :],
                               start=(kc == 0), stop=(kc == KC - 1))
            return ins
        P.op("pe", mm, reads=[s_sb, wt], writes=[ps])
        P.op("dve", lambda e, ps=ps, blk=blk: e.tensor_tensor(
            out=o_sb[:, blk * MB:(blk + 1) * MB], in0=ps[0:2, 0:MB],
            in1=b_sb[:, blk * MB:(blk + 1) * MB], op=ALU.add),
            reads=[ps, b_sb], writes=[o_sb])
    P.dma("sp", mo.ap(), o_sb[:], o_sb, reads=[o_sb])
    return P.finish()


def load_consts(P, names_shapes):
    out = {}
    for name, shape in names_shapes:
        d = P.dram(name, shape, F32, "ExternalInput")
        t = P.sb(name + "_sb", shape, F32)
        P.dma("sp", t[:], d.ap(), t, writes=[t])
        out[name] = t
    return out


def modulate_load(P, xT_d, sc1p, sh, hT, xbufs):
    for kc in range(KC):
        xb = xbufs[kc % len(xbufs)]
        P.dma("sp", xb[:], xT_d.ap()[kc * 128:(kc + 1) * 128, :], xb, writes=[xb])

        def f(e, kc=kc, xb=xb):
            e.activation(out=hT[:, kc, 0:LAT], in_=xb[:, 0:LAT], func=AF.Identity,
                         bias=sh[:, kc, 0:1], scale=sc1p[:, kc, 0:1])
            return e.activation(out=hT[:, kc, LAT:T], in_=xb[:, LAT:T], func=AF.Identity,
                                bias=sh[:, kc, 1:2], scale=sc1p[:, kc, 1:2])
        P.op("act", f, reads=[xb, sc1p, sh], writes=[hT])


def gemm_fm(P, hT, nk, w_d, col0, ncol, wbufs, ps3, widx):
    wt = wbufs[widx % len(wbufs)]
    w_ap = w_d if isinstance(w_d, bass.AP) else w_d.ap()
    src = w_ap[:, col0:col0 + ncol].rearrange("(kc p) n -> p kc n", p=128)
    P.dma("pool", wt[:, 0:nk, 0:ncol], src, wt, writes=[wt])

    def mm(e):
        ins = None
        for kc in range(nk):
            for ti, (t0, tn) in enumerate(TT):
                ins = e.matmul(ps3[ti][0:ncol, 0:tn], lhsT=wt[:, kc, 0:ncol], rhs=hT[:, kc, t0:t0 + tn],
                               start=(kc == 0), stop=(kc == nk - 1))
        return ins
    P.op("pe", mm, reads=[wt, hT], writes=list(ps3))


def make_rot(P, rot_d_name="rotm", n=128):
    d = P.dram(rot_d_name, [n, n], F32, "ExternalInput")
    t32 = P.sb(rot_d_name + "32", [n, n], F32)
    tb = P.sb(rot_d_name + "b", [n, n], BF16)
    P.dma("sp", t32[:], d.ap(), t32, writes=[t32])
    P.op("dve", lambda e: e.tensor_copy(out=tb[:], in_=t32[:]), reads=[t32], writes=[tb])
    return tb


def rot_matrix(n):
    half = n // 2
    m = np.zeros((n, n), np.float32)
    for j in range(half):
        m[j + half, j] = -1.0
        m[j, j + half] = 1.0
    return m


EV_IN = 9216


def build_even_a():
    P = Prog()
    xT_d = P.dram("xT", [D, T], F32, "ExternalInput")
    w_d = P.dram("w_in", [D, EV_IN], F32, "ExternalInput")
    QT_d = P.dram("QT", [32, 128, T], BF16, "ExternalOutput")
    KT_d = P.dram("KT", [20, 128, T], BF16, "ExternalOutput")
    VT_d = P.dram("VT", [20, 128, T], BF16, "ExternalOutput")
    C = load_consts(P, [("sc", [128, KC, 2]), ("sh", [128, KC, 2]), ("cosT", [128, T]),
                        ("sinT", [128, T]), ("gq", [128, 1]), ("gk", [128, 1])])
    rot = make_rot(P)
    ones = P.sb("ones", [128, 128], BF16)
    P.op("dve", lambda e: e.memset(ones[:], 1.0 / 128.0), writes=[ones])
    epsr = P.sb("epsr", [128, 1], F32)
    P.op("dve", lambda e: e.memset(epsr[:], RMS_EPS), writes=[epsr])
    sc1p = P.sb("sc1p", [128, KC, 2], F32)
    P.op("dve", lambda e: e.tensor_scalar(out=sc1p[:], in0=C["sc"][:], scalar1=1.0, scalar2=None,
                                          op0=ALU.add), reads=[C["sc"]], writes=[sc1p])
    hT = P.sb("hT", [128, KC, T], BF16)
    xbufs = [P.sb("xb%d" % i, [128, T], F32) for i in range(3)]
    import os
    if os.environ.get("K_STAGE") == "0":
        return P.finish()
    modulate_load(P, xT_d, sc1p, C["sh"], hT, xbufs)
    if os.environ.get("K_STAGE") == "1":
        return P.finish()
    wbufs = [P.sb("wb%d" % i, [128, KC, 128], BF16) for i in range(3)]
    obufs = [P.sb("ob%d" % i, [128, T], BF16) for i in range(3)]
    qb = [P.sb("qb%d" % i, [128, 512], BF16) for i in range(2)]
    sq = [P.sb("sq%d" % i, [128, 512], BF16) for i in range(2)]
    rs = [P.sb("rs%d" % i, [128, 512], F32) for i in range(2)]
    qn = [P.sb("qn%d" % i, [128, 512], F32) for i in range(2)]
    t1 = [P.sb("t1%d" % i, [128, 512], F32) for i in range(2)]
    t2 = [P.sb("t2%d" % i, [128, 512], F32) for i in range(2)]
    cosT, sinT = C["cosT"], C["sinT"]
    psA = [P.psum[0:3], P.psum[3:6]]
    ps_r, ps_s = P.psum[6], P.psum[7]
    plan = []
    for i in range(16):
        plan.append(("rope", QT_d, i, None))
    for i in range(16):
        plan.append(("rope", KT_d, i, None))
    for i in range(16):
        plan.append(("plain", VT_d, i, None))
    for i in range(16):
        plan.append(("rms", QT_d, 16 + i, C["gq"]))
    for i in range(4):
        plan.append(("rms", KT_d, 16 + i, C["gk"]))
    for i in range(4):
        plan.append(("plain", VT_d, 16 + i, None))
    cnt = 0
    import os
    plan = plan[:int(os.environ.get('K_PLAN', '999'))]
    for oc, (kind, od, oi, g) in enumerate(plan):
        ps3 = psA[oc % 2]
        gemm_fm(P, hT, KC, w_d, oc * 128, 128, wbufs, ps3, oc)
        if os.environ.get("K_STAGE") == "2":
            return P.finish()
        ob = obufs[oc % 3]
        for ti, (t0, tn) in enumerate(TT):
            ps = ps3[ti]
            j = cnt % 2
            cnt += 1
            if kind == "plain":
                P.op("act", lambda e, ps=ps, t0=t0, tn=tn, ob=ob: e.activation(
                    out=ob[:, t0:t0 + tn], in_=ps[:, 0:tn], func=AF.Copy), reads=[ps], writes=[ob])
                continue
            if kind == "rms":
                P.op("act", lambda e, ps=ps, tn=tn, j=j: e.activation(
                    out=sq[j][:, 0:tn], in_=ps[:, 0:tn], func=AF.Square), reads=[ps], writes=[sq[j]])
                P.op("pe", lambda e, tn=tn, j=j: e.matmul(ps_s[:, 0:tn], lhsT=ones[:], rhs=sq[j][:, 0:tn],
                                                          start=True, stop=True),
                     reads=[ones, sq[j]], writes=[ps_s])
                P.op("act", lambda e, tn=tn, j=j: e.activation(
                    out=rs[j][:, 0:tn], in_=ps_s[:, 0:tn], func=AF.Sqrt, bias=epsr[:, 0:1], scale=1.0),
                    reads=[ps_s, epsr], writes=[rs[j]])
                P.op("dve", lambda e, tn=tn, j=j: e.reciprocal(out=rs[j][:, 0:tn], in_=rs[j][:, 0:tn]),
                     reads=[rs[j]], writes=[rs[j]])
                P.op("act", lambda e, ps=ps, tn=tn, j=j: e.activation(
                    out=qn[j][:, 0:tn], in_=ps[:, 0:tn], func=AF.Copy), reads=[ps], writes=[qn[j]])
                P.op("dve", lambda e, tn=tn, j=j, g=g: e.scalar_tensor_tensor(
                    out=qn[j][:, 0:tn], in0=qn[j][:, 0:tn], scalar=g[:, 0:1], in1=rs[j][:, 0:tn],
                    op0=ALU.mult, op1=ALU.mult), reads=[qn[j], rs[j], g], writes=[qn[j]])
                src_t, src_ap = qn[j], qn[j]
            else:
                P.op("act", lambda e, ps=ps, tn=tn, j=j: e.activation(
                    out=qn[j][:, 0:tn], in_=ps[:, 0:tn], func=AF.Copy), reads=[ps], writes=[qn[j]])
                src_t = qn[j]
            P.op("act", lambda e, s=src_t, tn=tn, j=j: e.activation(
                out=qb[j][:, 0:tn], in_=s[:, 0:tn], func=AF.Copy), reads=[src_t], writes=[qb[j]])
            P.op("pe", lambda e, tn=tn, j=j: e.matmul(ps_r[:, 0:tn], lhsT=rot[:], rhs=qb[j][:, 0:tn],
                                                      start=True, stop=True),
                 reads=[rot, qb[j]], writes=[ps_r])
            if os.environ.get("K_STAGE") == "3":
                return P.finish()
            if os.environ.get("K_SKIPDVE") == "1":
                continue
            P.op("dve", lambda e, s=src_t, t0=t0, tn=tn, j=j: e.tensor_tensor(
                out=t1[j][:, 0:tn], in0=(sinT[:, t0:t0 + tn] if os.environ.get("K_NOPS") else s[:, 0:tn]), in1=cosT[:, t0:t0 + tn], op=ALU.mult),
                reads=[src_t, cosT], writes=[t1[j]])
            if os.environ.get("K_DVE") == "1":
                continue
            P.op("act", lambda e, tn=tn, j=j: e.activation(
                out=t2[j][:, 0:tn], in_=ps_r[:, 0:tn], func=AF.Copy), reads=[ps_r], writes=[t2[j]])
            P.op("dve", lambda e, t0=t0, tn=tn, j=j: e.tensor_tensor(
                out=t2[j][:, 0:tn], in0=t2[j][:, 0:tn], in1=sinT[:, t0:t0 + tn], op=ALU.mult),
                reads=[t2[j], sinT], writes=[t2[j]])
            if os.environ.get("K_DVE") == "2":
                continue
            P.op("dve", lambda e, t0=t0, tn=tn, j=j, ob=ob: e.tensor_tensor(
                out=ob[:, t0:t0 + tn], in0=t1[j][:, 0:tn], in1=t2[j][:, 0:tn], op=ALU.add),
                reads=[t1[j], t2[j]], writes=[ob])
        if os.environ.get("K_STAGE") == "4":
            return P.finish()
        P.dma("sp", od.ap()[oi], ob[:], ob, reads=[ob])
    return P.finish()


def rope_tables(core, rot_dim):
    idx = core * LAT + np.arange(LAT)
    row = (idx // GRID_W).astype(np.float32)
    col = (idx % GRID_W).astype(np.float32)
    quarter = rot_dim // 4
    inv = (10000.0 ** (-np.arange(quarter, dtype=np.float32) / quarter)).astype(np.float32)
    ang = np.concatenate([row[:, None] * inv, col[:, None] * inv], axis=-1)
    cos = np.ones((rot_dim, T), np.float32)
    sin = np.zeros((rot_dim, T), np.float32)
    c, s = np.cos(ang).T.astype(np.float32), np.sin(ang).T.astype(np.float32)
    h = rot_dim // 2
    cos[0:h, 0:LAT] = c
    cos[h:, 0:LAT] = c
    sin[0:h, 0:LAT] = s
    sin[h:, 0:LAT] = s
    return cos, sin


def fm(vec):
    return np.ascontiguousarray(vec.reshape(-1, 128).T)


def fm2(v_lat, v_ctx):
    return np.ascontiguousarray(np.stack([fm(v_lat), fm(v_ctx)], axis=-1))


def shard_tokens_T(x_lat, x_ctx, core):
    a = x_lat[core * LAT:(core + 1) * LAT]
    b = x_ctx[core * CT:(core + 1) * CT]
    return np.ascontiguousarray(np.concatenate([a, b], axis=0).T)


NKEY = SEQ + CTX
QTILES = [(0, 512, list(range(NKB))), (512, 512, list(range(NKB))), (LAT, CT, [NKB - 2, NKB - 1])]


def attn_unit(P, Qt, Kt, Vts, outs, Ptp, Spairs, psO, psL, onesb, scale, rl, of, cnt0, Qt2=None, Kt2=None,
              accL=None, onesf=None):
    cnt = cnt0
    NS = len(Spairs)
    LA = NS - 1
    for (t0, tn, kbs) in QTILES:
        npair = len(kbs) // 2

        def rec_qk(p, t0=t0, tn=tn, kbs=kbs, base=cnt):
            h, hA, hB = Spairs[(base + p) % NS]
            Pt = Ptp[(base + p) % NS]

            def qk(e):
                ins = None
                for half, hh in ((0, hA), (1, hB)):
                    kb = kbs[2 * p + half]
                    if Qt2 is None:
                        ins = e.matmul(hh[:, 0:tn], lhsT=Kt[:, kb * 128:(kb + 1) * 128], rhs=Qt[:, t0:t0 + tn],
                                       start=True, stop=True)
                    else:
                        e.matmul(hh[:, 0:tn], lhsT=Kt[:, kb * 128:(kb + 1) * 128], rhs=Qt[:, t0:t0 + tn],
                                 start=True, stop=False)
                        ins = e.matmul(hh[:, 0:tn], lhsT=Kt2[0:64, kb * 128:(kb + 1) * 128],
                                       rhs=Qt2[0:64, t0:t0 + tn], start=False, stop=True)
                return ins
            rd = [Kt, Qt] if Qt2 is None else [Kt, Qt, Kt2, Qt2]
            P.op("pe", qk, reads=rd, writes=[hA, hB])
            src = h[:, :].rearrange("p (b n) -> p b n", b=2)[:, :, 0:tn]
            P.op("act", lambda e: e.activation(out=Pt[:, :, 0:tn], in_=src, func=AF.Exp, scale=scale),
                 reads=[hA, hB], writes=[Pt])

        def rec_pv(p, tn=tn, kbs=kbs, base=cnt, npair=npair):
            Pt = Ptp[(base + p) % NS]
            first, last = (p == 0), (p == npair - 1)

            def pv(e):
                ins = None
                for half in (0, 1):
                    kb = kbs[2 * p + half]
                    st, sp = (first and half == 0), (last and half == 1)
                    for vi, Vt in enumerate(Vts):
                        ins = e.matmul(psO[vi][:, 0:tn], lhsT=Vt[:, kb, :], rhs=Pt[:, half, 0:tn], start=st, stop=sp)
                    if accL is None:
                        ins = e.matmul(psL[:, 0:tn], lhsT=onesb[:], rhs=Pt[:, half, 0:tn], start=st, stop=sp)
                return ins
            wr = list(psO[:len(Vts)]) + ([psL] if accL is None else [])
            P.op("pe", pv, reads=list(Vts) + [Pt, onesb], writes=wr)
            if accL is not None:
                if first:
                    P.op("dve", lambda e: e.tensor_copy(out=accL[:, :, 0:tn], in_=Pt[:, :, 0:tn]),
                         reads=[Pt], writes=[accL])
                else:
                    P.op("dve", lambda e: e.tensor_tensor(out=accL[:, :, 0:tn], in0=accL[:, :, 0:tn],
                                                          in1=Pt[:, :, 0:tn], op=ALU.add),
                         reads=[Pt, accL], writes=[accL])
                if last:
                    def lsum(e):
                        e.matmul(psL[:, 0:tn], lhsT=onesf[:], rhs=accL[:, 0, 0:tn], start=True, stop=False)
                        return e.matmul(psL[:, 0:tn], lhsT=onesf[:], rhs=accL[:, 1, 0:tn], start=False, stop=True)
                    P.op("pe", lsum, reads=[onesf, accL], writes=[psL])

        for p in range(npair + LA):
            if p < npair:
                rec_qk(p)
            if p >= LA:
                rec_pv(p - LA)
        cnt += npair
        P.op("act", lambda e, tn=tn: e.activation(out=rl[:, 0:tn], in_=psL[:, 0:tn], func=AF.Copy),
             reads=[psL], writes=[rl])
        P.op("dve", lambda e, tn=tn: e.reciprocal(out=rl[:, 0:tn], in_=rl[:, 0:tn]), reads=[rl], writes=[rl])
        for vi in range(len(Vts)):
            P.op("act", lambda e, vi=vi, tn=tn: e.activation(out=of[:, 0:tn], in_=psO[vi][:, 0:tn], func=AF.Copy),
                 reads=[psO[vi]], writes=[of])
            P.op("dve", lambda e, vi=vi, t0=t0, tn=tn: e.tensor_tensor(
                out=outs[vi][:, t0:t0 + tn], in0=of[:, 0:tn], in1=rl[:, 0:tn], op=ALU.mult),
                reads=[of, rl], writes=[outs[vi]])
    return cnt


def build_even_b1(lambda_init):
    P = Prog()
    QT_d = P.dram("QT", [32, 128, T], BF16, "ExternalInput")
    KT_d = P.dram("KTall", [20, 128, NKEY], BF16, "ExternalInput")
    V_d = P.dram("Vall", [20, 128, NKB, 128], BF16, "ExternalInput")
    AT_d = P.dram("attnT", [32, 128, T], BF16, "ExternalOutput")
    C = load_consts(P, [("lamv", [128, 4]), ("subg", [128, 2])])
    scale = 1.0 / math.sqrt(128.0)
    onesb = P.sb("onesb", [128, 128], BF16)
    P.op("dve", lambda e: e.memset(onesb[:], 1.0), writes=[onesb])
    ones256 = P.sb("ones256", [128, 128], BF16)
    P.op("dve", lambda e: e.memset(ones256[:], 1.0 / 256.0), writes=[ones256])
    onesf = P.sb("onesf", [128, 128], F32)
    P.op("dve", lambda e: e.memset(onesf[:], 1.0), writes=[onesf])
    epsr = P.sb("epsr", [128, 1], F32)
    P.op("dve", lambda e: e.memset(epsr[:], RMS_EPS), writes=[epsr])
    ps_lam, ps_ss = P.psum[7], P.psum[7]
    prods = P.sb("prods", [128, 2], F32)
    ex = P.sb("ex", [128, 2], F32)
    neglam = P.sb("neglam", [128, 1], F32)
    lamv = C["lamv"]
    P.op("dve", lambda e: e.tensor_tensor(out=prods[:, 0:1], in0=lamv[:, 0:1], in1=lamv[:, 1:2], op=ALU.mult),
         reads=[lamv], writes=[prods])
    P.op("dve", lambda e: e.tensor_tensor(out=prods[:, 1:2], in0=lamv[:, 2:3], in1=lamv[:, 3:4], op=ALU.mult),
         reads=[lamv, prods], writes=[prods])
    P.op("pe", lambda e: e.matmul(ps_lam[:, 0:2], lhsT=onesf[:], rhs=prods[:], start=True, stop=True),
         reads=[onesf, prods], writes=[ps_lam])
    P.op("act", lambda e: e.activation(out=ex[:], in_=ps_lam[:, 0:2], func=AF.Exp), reads=[ps_lam], writes=[ex])
    P.op("dve", lambda e: e.tensor_tensor(out=neglam[:], in0=ex[:, 1:2], in1=ex[:, 0:1], op=ALU.subtract),
         reads=[ex], writes=[neglam])
    P.op("dve", lambda e: e.tensor_scalar(out=neglam[:], in0=neglam[:], scalar1=-float(lambda_init), scalar2=None,
                                          op0=ALU.add), reads=[neglam], writes=[neglam])
    gsc = P.sb("gsc", [128, 2], F32)
    P.op("dve", lambda e: e.tensor_scalar(out=gsc[:], in0=C["subg"][:], scalar1=float(1.0 - lambda_init),
                                          scalar2=None, op0=ALU.mult), reads=[C["subg"]], writes=[gsc])
    Kb = [P.sb("Kb%d" % i, [128, NKEY], BF16) for i in range(4)]
    Vb = [P.sb("Vb%d" % i, [128, NKB, 128], BF16) for i in range(4)]
    Qb = [P.sb("Qb%d" % i, [128, T], BF16) for i in range(4)]
    Pb = [P.sb("Pb%d" % i, [128, 2, 512], BF16) for i in range(2)]
    A1 = [P.sb("A1%d" % i, [128, T], F32) for i in range(2)]
    A2 = [P.sb("A2%d" % i, [128, T], F32) for i in range(2)]
    sqb = [P.sb("sqb%d" % i, [128, 512], BF16) for i in range(2)]
    rl = P.sb("rl", [128, 512], F32)
    of = P.sb("of", [128, 512], F32)
    rs = P.sb("rs", [128, 512], F32)
    accL = P.sb("accL", [128, 2, 512], F32)
    obs = [P.sb("obs%d" % i, [128, T], BF16) for i in range(4)]
    psS = [(P.pp[0], P.psum[0], P.psum[1]), (P.pp[1], P.psum[2], P.psum[3])]
    psO, psL = P.psum[4:6], P.psum[6]
    cnt = 0
    import os
    nheads = int(os.environ.get("K_NH", "8"))
    ngqa = int(os.environ.get("K_NG", "16"))
    for h in range(nheads):
        ia, ib = (2 * h) % 4, (2 * h + 1) % 4
        for ch, i4 in ((2 * h, ia), (2 * h + 1, ib)):
            P.dma("sp", Qb[i4][:], QT_d.ap()[ch], Qb[i4], writes=[Qb[i4]])
            P.dma("sp", Kb[i4][:], KT_d.ap()[ch], Kb[i4], writes=[Kb[i4]])
            P.dma("sp", Vb[i4][:], V_d.ap()[ch], Vb[i4], writes=[Vb[i4]])
        Vts = [Vb[ia], Vb[ib]]
        cnt = attn_unit(P, Qb[ia], Kb[ia], Vts, A1, Pb, psS, psO, psL, onesb, scale, rl, of, cnt,
                        accL=accL, onesf=onesf)
        cnt = attn_unit(P, Qb[ib], Kb[ib], Vts, A2, Pb, psS, psO, psL, onesb, scale, rl, of, cnt,
                        accL=accL, onesf=onesf)
        o0, o1 = obs[(2 * h) % 4], obs[(2 * h + 1) % 4]
        for vi in range(2):
            P.op("dve", lambda e, vi=vi: e.scalar_tensor_tensor(
                out=A1[vi][:], in0=A2[vi][:], scalar=neglam[:, 0:1], in1=A1[vi][:], op0=ALU.mult, op1=ALU.add),
                reads=[A2[vi], A1[vi], neglam], writes=[A1[vi]])
        for (t0, tn) in TT:
            for vi in range(2):
                P.op("act", lambda e, vi=vi, t0=t0, tn=tn: e.activation(
                    out=sqb[vi][:, 0:tn], in_=A1[vi][:, t0:t0 + tn], func=AF.Square), reads=[A1[vi]], writes=[sqb[vi]])

            def ssmm(e, tn=tn):
                e.matmul(ps_ss[:, 0:tn], lhsT=ones256[:], rhs=sqb[0][:, 0:tn], start=True, stop=False)
                return e.matmul(ps_ss[:, 0:tn], lhsT=ones256[:], rhs=sqb[1][:, 0:tn], start=False, stop=True)
            P.op("pe", ssmm, reads=[ones256, sqb[0], sqb[1]], writes=[ps_ss])
            P.op("act", lambda e, tn=tn: e.activation(out=rs[:, 0:tn], in_=ps_ss[:, 0:tn], func=AF.Sqrt,
                                                      bias=epsr[:, 0:1], scale=1.0), reads=[ps_ss, epsr], writes=[rs])
            P.op("dve", lambda e, tn=tn: e.reciprocal(out=rs[:, 0:tn], in_=rs[:, 0:tn]), reads=[rs], writes=[rs])
            for vi, ob in ((0, o0), (1, o1)):
                P.op("dve", lambda e, vi=vi, ob=ob, t0=t0, tn=tn: e.scalar_tensor_tensor(
                    out=ob[:, t0:t0 + tn], in0=A1[vi][:, t0:t0 + tn], scalar=gsc[:, vi:vi + 1], in1=rs[:, 0:tn],
                    op0=ALU.mult, op1=ALU.mult), reads=[A1[vi], gsc, rs], writes=[ob])
        P.dma("sp", AT_d.ap()[2 * h], o0[:], o0, reads=[o0])
        P.dma("sp", AT_d.ap()[2 * h + 1], o1[:], o1, reads=[o1])
    for g in range(ngqa):
        kv = g // 4
        if g % 4 == 0:
            P.dma("sp", Kb[kv % 4][:], KT_d.ap()[16 + kv], Kb[kv % 4], writes=[Kb[kv % 4]])
            P.dma("sp", Vb[kv % 4][:], V_d.ap()[16 + kv], Vb[kv % 4], writes=[Vb[kv % 4]])
        P.dma("sp", Qb[g % 4][:], QT_d.ap()[16 + g], Qb[g % 4], writes=[Qb[g % 4]])
        cnt = attn_unit(P, Qb[g % 4], Kb[kv % 4], [Vb[kv % 4]], [A1[g % 2]], Pb, psS, psO, psL, onesb, scale,
                        rl, of, cnt)
        ob = obs[g % 4]
        P.op("act", lambda e, g=g, ob=ob: e.activation(out=ob[:], in_=A1[g % 2][:], func=AF.Copy),
             reads=[A1[g % 2]], writes=[ob])
        P.dma("sp", AT_d.ap()[16 + g], ob[:], ob, reads=[ob])
    return P.finish()


NEXP = 16
DEXP = 384
TOK_TILES = [(i * 128, 128) for i in range(LAT // 128)] + [(LAT, CT)]


def ln_stats_accum(P, yb, t0, tn, ti, oc, noc, S1, S2, ybf, ysq, onesb):
    P.op("act", lambda e: e.activation(out=ybf[:, 0:tn], in_=yb[:, t0:t0 + tn], func=AF.Copy),
         reads=[yb], writes=[ybf])
    P.op("act", lambda e: e.activation(out=ysq[:, 0:tn], in_=yb[:, t0:t0 + tn], func=AF.Square),
         reads=[yb], writes=[ysq])
    s1t, s1c = S1[ti]
    s2t, s2c = S2[ti]

    def mm(e):
        e.matmul(s1t[:, s1c:s1c + tn], lhsT=onesb[:], rhs=ybf[:, 0:tn], start=(oc == 0), stop=(oc == noc - 1))
        return e.matmul(s2t[:, s2c:s2c + tn], lhsT=onesb[:], rhs=ysq[:, 0:tn], start=(oc == 0), stop=(oc == noc - 1))
    P.op("pe", mm, reads=[onesb, ybf, ysq], writes=[s1t, s2t])


def ln_finalize(P, S1, S2, mu, rstd, nmr, tmp, epsl):
    for ti, (t0, tn) in enumerate(TT):
        s1t, s1c = S1[ti]
        s2t, s2c = S2[ti]
        P.op("act", lambda e, s1t=s1t, s1c=s1c, t0=t0, tn=tn: e.activation(
            out=mu[:, t0:t0 + tn], in_=s1t[:, s1c:s1c + tn], func=AF.Copy, scale=1.0 / D), reads=[s1t], writes=[mu])
        P.op("act", lambda e, s2t=s2t, s2c=s2c, t0=t0, tn=tn: e.activation(
            out=rstd[:, t0:t0 + tn], in_=s2t[:, s2c:s2c + tn], func=AF.Copy, scale=1.0 / D), reads=[s2t], writes=[rstd])
    P.op("dve", lambda e: e.tensor_tensor(out=tmp[:], in0=mu[:], in1=mu[:], op=ALU.mult), reads=[mu], writes=[tmp])
    P.op("dve", lambda e: e.tensor_tensor(out=rstd[:], in0=rstd[:], in1=tmp[:], op=ALU.subtract),
         reads=[rstd, tmp], writes=[rstd])
    P.op("act", lambda e: e.activation(out=rstd[:], in_=rstd[:], func=AF.Sqrt, bias=epsl[:, 0:1], scale=1.0),
         reads=[rstd, epsl], writes=[rstd])
    P.op("dve", lambda e: e.reciprocal(out=rstd[:], in_=rstd[:]), reads=[rstd], writes=[rstd])
    P.op("dve", lambda e: e.tensor_tensor(out=nmr[:], in0=mu[:], in1=rstd[:], op=ALU.mult), reads=[mu, rstd], writes=[nmr])


def build_post():
    P = Prog()
    AT_d = P.dram("attnT", [32, 128, T], BF16, "ExternalInput")
    xT_d = P.dram("xT", [D, T], F32, "ExternalInput")
    wo_d = P.dram("w_out", [D, D], F32, "ExternalInput")
    wg_d = P.dram("w_gate", [NEXP, D, DEXP], F32, "ExternalInput")
    wu_d = P.dram("w_up", [NEXP, D, DEXP], F32, "ExternalInput")
    wd_d = P.dram("w_down", [NEXP * DEXP, D], F32, "ExternalInput")
    wgr_d = P.dram("wgr", [D, 20], F32, "ExternalInput")
    xo_d = P.dram("xT_new", [D, T], F32, "ExternalOutput")
    y1_d = P.dram("y1T", [D, T], F32, "Internal")
    x1_d = P.dram("x1T", [D, T], F32, "Internal")
    fp_d = P.dram("fpart", [D, T], F32, "Internal")
    y2_d = P.dram("y2T", [D, T], F32, "Internal")
    C = load_consts(P, [("g1", [128, KC, 2]), ("sc2", [128, KC, 2]), ("sh2", [128, KC, 2]), ("g2", [128, KC, 2]),
                        ("ln1g", [128, KC]), ("ln1b", [128, KC]), ("ln2g", [128, KC]), ("ln2b", [128, KC]),
                        ("bgr", [128, 20]), ("identf", [128, 128]), ("selm", [16, NEXP * 128])])
    onesb = P.sb("onesb", [128, 128], BF16)
    P.op("dve", lambda e: e.memset(onesb[:], 1.0), writes=[onesb])
    epsl = P.sb("epsl", [128, 1], F32)
    P.op("dve", lambda e: e.memset(epsl[:], LN_EPS), writes=[epsl])
    sc2p = P.sb("sc2p", [128, KC, 2], F32)
    P.op("dve", lambda e: e.tensor_scalar(out=sc2p[:], in0=C["sc2"][:], scalar1=1.0, scalar2=None, op0=ALU.add),
         reads=[C["sc2"]], writes=[sc2p])
    mu = P.sb("mu", [128, T], F32)
    rstd = P.sb("rstd", [128, T], F32)
    nmr = P.sb("nmr", [128, T], F32)
    tmpT = P.sb("tmpT", [128, T], F32)
    wbufs = [P.sb("wb%d" % i, [128, KC, 128], BF16) for i in range(3)]
    xbufs = [P.sb("xb%d" % i, [128, T], F32) for i in range(2)]
    ybufs = [P.sb("yb%d" % i, [128, T], F32) for i in range(2)]
    of = [P.sb("of0", [128, 512], F32)] * 2
    ybf = P.sb("ybf", [128, 512], BF16)
    ysq = P.sb("ysq", [128, 512], BF16)
    ps3 = P.psum[0:3]
    S1 = [(P.psum[3], 0), (P.psum[4], 0), (P.psum[7], 0)]
    S2 = [(P.psum[5], 0), (P.psum[6], 0), (P.psum[7], 64)]
    h2T = P.sb("h2T", [128, KC, T], BF16)
    m_phase = P.mark()

    def residual_epilogue(oc, gmod, src_x_d, dst_y_d, extra_d=None):
        xb = xbufs[oc % 2]
        yb = ybufs[oc % 2]
        P.dma("sp", xb[:], src_x_d.ap()[oc * 128:(oc + 1) * 128, :], xb, writes=[xb])
        P.op("act", lambda e: e.activation(out=xb[:], in_=xb[:], func=AF.Copy, scale=float(ALPHA)),
             reads=[xb], writes=[xb])
        if extra_d is not None:
            P.dma("sp", yb[:], extra_d.ap()[oc * 128:(oc + 1) * 128, :], yb, writes=[yb])
        for ti, (t0, tn) in enumerate(TT):
            o = of[ti % 2]
            P.op("act", lambda e, ti=ti, tn=tn, o=o: e.activation(out=o[:, 0:tn], in_=ps3[ti][:, 0:tn], func=AF.Copy),
                 reads=[ps3[ti]], writes=[o])
            if extra_d is not None:
                P.op("dve", lambda e, t0=t0, tn=tn, o=o: e.tensor_tensor(
                    out=o[:, 0:tn], in0=o[:, 0:tn], in1=yb[:, t0:t0 + tn], op=ALU.add), reads=[o, yb], writes=[o])
            col = 0 if ti < 2 else 1
            P.op("dve", lambda e, t0=t0, tn=tn, o=o, col=col: e.scalar_tensor_tensor(
                out=yb[:, t0:t0 + tn], in0=o[:, 0:tn], scalar=gmod[:, oc, col:col + 1], in1=xb[:, t0:t0 + tn],
                op0=ALU.mult, op1=ALU.add), reads=[o, gmod, xb], writes=[yb])
            ln_stats_accum(P, yb, t0, tn, ti, oc, KC, S1, S2, ybf, ysq, onesb)
        P.dma("sp", dst_y_d.ap()[oc * 128:(oc + 1) * 128, :], yb[:], yb, reads=[yb])

    def ln_apply(src_d, g, b, dst_d, with_h2):
        for oc in range(KC):
            yb = ybufs[oc % 2]
            xb = xbufs[oc % 2]
            P.dma("sp", yb[:], src_d.ap()[oc * 128:(oc + 1) * 128, :], yb, writes=[yb])
            P.op("dve", lambda e, yb=yb: e.tensor_tensor(out=yb[:], in0=yb[:], in1=rstd[:], op=ALU.mult),
                 reads=[yb, rstd], writes=[yb])
            P.op("dve", lambda e, yb=yb: e.tensor_tensor(out=yb[:], in0=yb[:], in1=nmr[:], op=ALU.subtract),
                 reads=[yb, nmr], writes=[yb])
            P.op("act", lambda e, yb=yb, xb=xb, oc=oc: e.activation(
                out=xb[:], in_=yb[:], func=AF.Identity, bias=b[:, oc:oc + 1], scale=g[:, oc:oc + 1]),
                reads=[yb, g, b], writes=[xb])
            P.dma("sp", dst_d.ap()[oc * 128:(oc + 1) * 128, :], xb[:], xb, reads=[xb])
            if with_h2:
                def f(e, xb=xb, oc=oc):
                    e.activation(out=h2T[:, oc, 0:LAT], in_=xb[:, 0:LAT], func=AF.Identity,
                                 bias=C["sh2"][:, oc, 0:1], scale=sc2p[:, oc, 0:1])
                    return e.activation(out=h2T[:, oc, LAT:T], in_=xb[:, LAT:T], func=AF.Identity,
                                        bias=C["sh2"][:, oc, 1:2], scale=sc2p[:, oc, 1:2])
                P.op("act", f, reads=[xb, sc2p, C["sh2"]], writes=[h2T])

    aT = P.sb("aT", [128, KC, T], BF16)
    for ch in range(32):
        P.dma("sp", aT[:, ch, :], AT_d.ap()[ch], aT, writes=[aT])
    for oc in range(KC):
        gemm_fm(P, aT, KC, wo_d, oc * 128, 128, wbufs, ps3, oc)
        residual_epilogue(oc, C["g1"], xT_d, y1_d)
    ln_finalize(P, S1, S2, mu, rstd, nmr, tmpT, epsl)
    P.fence()
    P.release(m_phase)
    ln_apply(y1_d, C["ln1g"], C["ln1b"], x1_d, True)
    P.fence()
    wgrb = P.sb("wgrb", [128, KC, 20], BF16)
    P.dma("pool", wgrb[:], wgr_d.ap().rearrange("(kc p) n -> p kc n", p=128), wgrb, writes=[wgrb])
    gT = P.sb("gT", [16, T], F32)
    lg = P.sb("lg", [128, 20], F32)
    sm = {n: P.sb("r_" + n, [128, w], F32) for n, w in (
        ("gmax", 1), ("ngmax", 1), ("ohg", 4), ("ge", 4), ("gsum", 1), ("gw", 1), ("el", 4), ("m1", 1), ("oh1", 4),
        ("el2", 4), ("m2", 1), ("oh2", 4), ("d", 1), ("e2", 1), ("den", 1), ("p1", 1), ("p2", 1), ("w1", 1),
        ("w2", 1), ("gin", 4), ("gates", 16))}
    ps_l, ps_t = P.psum[0], P.psum[1]
    AX = mybir.AxisListType.X
    for (t0, tn) in TOK_TILES:
        pr = slice(0, tn)

        def lmm(e, t0=t0, tn=tn):
            ins = None
            for kc in range(KC):
                ins = e.matmul(ps_l[0:tn, 0:20], lhsT=h2T[:, kc, t0:t0 + tn], rhs=wgrb[:, kc, :],
                               start=(kc == 0), stop=(kc == KC - 1))
            return ins
        P.op("pe", lmm, reads=[h2T, wgrb], writes=[ps_l])
        P.op("act", lambda e, pr=pr: e.activation(out=lg[pr, :], in_=ps_l[pr, 0:20], func=AF.Copy),
             reads=[ps_l], writes=[lg])
        P.op("dve", lambda e, pr=pr: e.tensor_tensor(out=lg[pr, :], in0=lg[pr, :], in1=C["bgr"][pr, :], op=ALU.add),
             reads=[lg, C["bgr"]], writes=[lg])

        def dv(fn, reads, writes):
            P.op("dve", fn, reads=[sm[r] if isinstance(r, str) else r for r in reads],
                 writes=[sm[w] for w in writes])
        dv(lambda e, pr=pr: e.reduce_max(out=sm["gmax"][pr, :], in_=lg[pr, 0:4], axis=AX), [lg], ["gmax"])
        dv(lambda e, pr=pr: e.tensor_scalar(out=sm["ohg"][pr, :], in0=lg[pr, 0:4], scalar1=sm["gmax"][pr, 0:1],
                                            scalar2=None, op0=ALU.is_ge), [lg, "gmax"], ["ohg"])
        dv(lambda e, pr=pr: e.tensor_scalar(out=sm["ngmax"][pr, :], in0=sm["gmax"][pr, :], scalar1=-1.0,
                                            scalar2=None, op0=ALU.mult), ["gmax"], ["ngmax"])
        P.op("act", lambda e, pr=pr: e.activation(out=sm["ge"][pr, :], in_=lg[pr, 0:4], func=AF.Exp,
                                                  bias=sm["ngmax"][pr, 0:1], scale=1.0),
             reads=[lg, sm["ngmax"]], writes=[sm["ge"]])
        dv(lambda e, pr=pr: e.reduce_sum(out=sm["gsum"][pr, :], in_=sm["ge"][pr, :], axis=AX), ["ge"], ["gsum"])
        dv(lambda e, pr=pr: e.reciprocal(out=sm["gw"][pr, :], in_=sm["gsum"][pr, :]), ["gsum"], ["gw"])
        dv(lambda e, pr=pr: e.tensor_scalar(out=sm["el"][pr, :], in0=lg[pr, 4:8], scalar1=sm["ohg"][pr, 0:1],
                                            scalar2=None, op0=ALU.mult), [lg, "ohg"], ["el"])
        for g in range(1, 4):
            dv(lambda e, pr=pr, g=g: e.scalar_tensor_tensor(
                out=sm["el"][pr, :], in0=lg[pr, 4 + 4 * g:8 + 4 * g], scalar=sm["ohg"][pr, g:g + 1],
                in1=sm["el"][pr, :], op0=ALU.mult, op1=ALU.add), [lg, "ohg", "el"], ["el"])
        dv(lambda e, pr=pr: e.reduce_max(out=sm["m1"][pr, :], in_=sm["el"][pr, :], axis=AX), ["el"], ["m1"])
        dv(lambda e, pr=pr: e.tensor_scalar(out=sm["oh1"][pr, :], in0=sm["el"][pr, :], scalar1=sm["m1"][pr, 0:1],
                                            scalar2=None, op0=ALU.is_ge), ["el", "m1"], ["oh1"])
        dv(lambda e, pr=pr: e.scalar_tensor_tensor(out=sm["el2"][pr, :], in0=sm["oh1"][pr, :], scalar=-1e30,
                                                   in1=sm["el"][pr, :], op0=ALU.mult, op1=ALU.add),
           ["oh1", "el"], ["el2"])
        dv(lambda e, pr=pr: e.reduce_max(out=sm["m2"][pr, :], in_=sm["el2"][pr, :], axis=AX), ["el2"], ["m2"])
        dv(lambda e, pr=pr: e.tensor_scalar(out=sm["oh2"][pr, :], in0=sm["el2"][pr, :], scalar1=sm["m2"][pr, 0:1],
                                            scalar2=None, op0=ALU.is_ge), ["el2", "m2"], ["oh2"])
        dv(lambda e, pr=pr: e.tensor_tensor(out=sm["d"][pr, :], in0=sm["m2"][pr, :], in1=sm["m1"][pr, :],
                                            op=ALU.subtract), ["m2", "m1"], ["d"])
        P.op("act", lambda e, pr=pr: e.activation(out=sm["e2"][pr, :], in_=sm["d"][pr, :], func=AF.Exp),
             reads=[sm["d"]], writes=[sm["e2"]])
        dv(lambda e, pr=pr: e.tensor_scalar(out=sm["den"][pr, :], in0=sm["e2"][pr, :], scalar1=1.0, scalar2=None,
                                            op0=ALU.add), ["e2"], ["den"])
        dv(lambda e, pr=pr: e.reciprocal(out=sm["p1"][pr, :], in_=sm["den"][pr, :]), ["den"], ["p1"])
        dv(lambda e, pr=pr: e.tensor_tensor(out=sm["p2"][pr, :], in0=sm["e2"][pr, :], in1=sm["p1"][pr, :],
                                            op=ALU.mult), ["e2", "p1"], ["p2"])
        dv(lambda e, pr=pr: e.tensor_tensor(out=sm["w1"][pr, :], in0=sm["p1"][pr, :], in1=sm["gw"][pr, :],
                                            op=ALU.mult), ["p1", "gw"], ["w1"])
        dv(lambda e, pr=pr: e.tensor_tensor(out=sm["w2"][pr, :], in0=sm["p2"][pr, :], in1=sm["gw"][pr, :],
                                            op=ALU.mult), ["p2", "gw"], ["w2"])
        dv(lambda e, pr=pr: e.tensor_scalar(out=sm["gin"][pr, :], in0=sm["oh1"][pr, :], scalar1=sm["w1"][pr, 0:1],
                                            scalar2=None, op0=ALU.mult), ["oh1", "w1"], ["gin"])
        dv(lambda e, pr=pr: e.scalar_tensor_tensor(out=sm["gin"][pr, :], in0=sm["oh2"][pr, :],
                                                   scalar=sm["w2"][pr, 0:1], in1=sm["gin"][pr, :],
                                                   op0=ALU.mult, op1=ALU.add), ["oh2", "w2", "gin"], ["gin"])
        for g in range(4):
            dv(lambda e, pr=pr, g=g: e.tensor_scalar(out=sm["gates"][pr, 4 * g:4 * g + 4], in0=sm["gin"][pr, :],
                                                     scalar1=sm["ohg"][pr, g:g + 1], scalar2=None, op0=ALU.mult),
               ["gin", "ohg", "gates"], ["gates"])
        P.op("pe", lambda e, pr=pr, tn=tn: e.matmul(ps_t[0:16, 0:tn], lhsT=sm["gates"][pr, :],
                                                    rhs=C["identf"][pr, 0:tn], start=True, stop=True),
             reads=[sm["gates"], C["identf"]], writes=[ps_t])
        P.op("act", lambda e, t0=t0, tn=tn: e.activation(out=gT[:, t0:t0 + tn], in_=ps_t[0:16, 0:tn], func=AF.Copy),
             reads=[ps_t], writes=[gT])
    hidT = P.sb("hidT", [128, 24, T], BF16)
    gbc = P.sb("gbc", [128, T], F32)
    sg = [P.sb("sg%d" % i, [128, 512], F32) for i in range(2)]
    uu = [P.sb("uu0", [128, 512], F32)] * 2
    psG, psU, ps_b = P.psum[0:3], P.psum[3:6], P.psum[6]
    widx = 0
    for half in range(2):
        for el in range(8):
            ex = half * 8 + el
            for ti, (t0, tn) in enumerate(TT):
                P.op("pe", lambda e, ex=ex, t0=t0, tn=tn: e.matmul(
                    ps_b[:, 0:tn], lhsT=C["selm"][:, ex * 128:(ex + 1) * 128], rhs=gT[:, t0:t0 + tn],
                    start=True, stop=True), reads=[C["selm"], gT], writes=[ps_b])
                P.op("act", lambda e, t0=t0, tn=tn: e.activation(out=gbc[:, t0:t0 + tn], in_=ps_b[:, 0:tn], func=AF.Copy),
                     reads=[ps_b], writes=[gbc])
            for fc in range(3):
                gemm_fm(P, h2T, KC, wg_d.ap()[ex], fc * 128, 128, wbufs, psG, widx)
                widx += 1
                gemm_fm(P, h2T, KC, wu_d.ap()[ex], fc * 128, 128, wbufs, psU, widx)
                widx += 1
                for ti, (t0, tn) in enumerate(TT):
                    j = ti % 2
                    P.op("act", lambda e, ti=ti, tn=tn, j=j: e.activation(out=sg[j][:, 0:tn], in_=psG[ti][:, 0:tn], func=AF.Silu),
                         reads=[psG[ti]], writes=[sg[j]])
                    P.op("act", lambda e, ti=ti, tn=tn, j=j: e.activation(out=uu[j][:, 0:tn], in_=psU[ti][:, 0:tn], func=AF.Copy),
                         reads=[psU[ti]], writes=[uu[j]])
                    P.op("dve", lambda e, tn=tn, j=j: e.tensor_tensor(out=sg[j][:, 0:tn], in0=sg[j][:, 0:tn], in1=uu[j][:, 0:tn], op=ALU.mult),
                         reads=[sg[j], uu[j]], writes=[sg[j]])
                    P.op("dve", lambda e, t0=t0, tn=tn, j=j, el=el, fc=fc: e.tensor_tensor(
                        out=hidT[:, el * 3 + fc, t0:t0 + tn], in0=sg[j][:, 0:tn], in1=gbc[:, t0:t0 + tn], op=ALU.mult),
                        reads=[sg[j], gbc], writes=[hidT])
        wd_half = wd_d.ap()[half * 8 * DEXP:(half + 1) * 8 * DEXP, :]
        for oc in range(KC):
            gemm_fm(P, hidT, 24, wd_half, oc * 128, 128, wbufs, ps3, widx)
            widx += 1
            if half == 0:
                yb = ybufs[oc % 2]
                for ti, (t0, tn) in enumerate(TT):
                    P.op("act", lambda e, ti=ti, t0=t0, tn=tn, yb=yb: e.activation(
                        out=yb[:, t0:t0 + tn], in_=ps3[ti][:, 0:tn], func=AF.Copy), reads=[ps3[ti]], writes=[yb])
                P.dma("sp", fp_d.ap()[oc * 128:(oc + 1) * 128, :], yb[:], yb, reads=[yb])
            else:
                residual_epilogue(oc, C["g2"], x1_d, y2_d, extra_d=fp_d)
        P.fence()
    ln_finalize(P, S1, S2, mu, rstd, nmr, tmpT, epsl)
    P.fence()
    ln_apply(y2_d, C["ln2g"], C["ln2b"], xo_d, False)
    return P.finish()


def sel_matrix():
    m = np.zeros((16, NEXP, 128), np.float32)
    for e in range(NEXP):
        m[e, e, :] = 1.0
    return np.ascontiguousarray(m.reshape(16, NEXP * 128))


OD_IN = 1600
NH = 32


def rope_tile(P, ps, nrow, t0, tn, j, qn, qb, t1, t2, rot, cosT, sinT, ps_r, ob):
    pr = slice(0, nrow)
    P.op("act", lambda e: e.activation(out=qn[j][pr, 0:tn], in_=ps[pr, 0:tn], func=AF.Copy), reads=[ps], writes=[qn[j]])
    P.op("act", lambda e: e.activation(out=qb[j][pr, 0:tn], in_=qn[j][pr, 0:tn], func=AF.Copy), reads=[qn[j]], writes=[qb[j]])
    P.op("pe", lambda e: e.matmul(ps_r[pr, 0:tn], lhsT=rot[pr, 0:nrow], rhs=qb[j][pr, 0:tn], start=True, stop=True),
         reads=[rot, qb[j]], writes=[ps_r])
    P.op("act", lambda e: e.activation(out=t2[j][pr, 0:tn], in_=ps_r[pr, 0:tn], func=AF.Copy), reads=[ps_r], writes=[t2[j]])
    P.op("dve", lambda e: e.tensor_tensor(out=t2[j][pr, 0:tn], in0=t2[j][pr, 0:tn], in1=sinT[pr, t0:t0 + tn], op=ALU.mult),
         reads=[t2[j], sinT], writes=[t2[j]])
    P.op("dve", lambda e: e.tensor_tensor(out=t1[j][pr, 0:tn], in0=qn[j][pr, 0:tn], in1=cosT[pr, t0:t0 + tn], op=ALU.mult),
         reads=[qn[j], cosT], writes=[t1[j]])
    P.op("dve", lambda e: e.tensor_tensor(out=ob[pr, t0:t0 + tn], in0=t1[j][pr, 0:tn], in1=t2[j][pr, 0:tn], op=ALU.add),
         reads=[t1[j], t2[j]], writes=[ob])


def build_odd_a():
    P = Prog()
    xT_d = P.dram("xT", [D, T], F32, "ExternalInput")
    w_d = P.dram("w_in", [D, OD_IN], F32, "ExternalInput")
    wuq_d = P.dram("w_uq", [1024, NH * 192], F32, "ExternalInput")
    wukv_d = P.dram("w_ukv", [512, NH * 256], F32, "ExternalInput")
    QN_d = P.dram("QN", [NH, 128, T], BF16, "ExternalOutput")
    QR_d = P.dram("QR", [NH, 64, T], BF16, "ExternalOutput")
    KN_d = P.dram("KN", [NH, 128, T], BF16, "ExternalOutput")
    KR_d = P.dram("KR", [64, T], BF16, "ExternalOutput")
    VT_d = P.dram("VT", [NH, 128, T], BF16, "ExternalOutput")
    C = load_consts(P, [("sc", [128, KC, 2]), ("sh", [128, KC, 2]), ("cosT", [64, T]), ("sinT", [64, T]),
                        ("gqn", [128, 8]), ("gkvn", [128, 4])])
    rot = make_rot(P, "rotm", 64)
    onesq = P.sb("onesq", [128, 128], BF16)
    P.op("dve", lambda e: e.memset(onesq[:], 1.0 / 1024.0), writes=[onesq])
    onesk = P.sb("onesk", [128, 128], BF16)
    P.op("dve", lambda e: e.memset(onesk[:], 1.0 / 512.0), writes=[onesk])
    epsr = P.sb("epsr", [128, 1], F32)
    P.op("dve", lambda e: e.memset(epsr[:], RMS_EPS), writes=[epsr])
    sc1p = P.sb("sc1p", [128, KC, 2], F32)
    P.op("dve", lambda e: e.tensor_scalar(out=sc1p[:], in0=C["sc"][:], scalar1=1.0, scalar2=None, op0=ALU.add),
         reads=[C["sc"]], writes=[sc1p])
    cq32 = P.sb("cq32", [128, 8, T], F32)
    ckv32 = P.sb("ckv32", [128, 4, T], F32)
    rsq = P.sb("rsq", [128, T], F32)
    rskv = P.sb("rskv", [128, T], F32)
    wbufs = [P.sb("wb%d" % i, [128, KC, 128], BF16) for i in range(3)]
    obufs = [P.sb("ob%d" % i, [128, T], BF16) for i in range(3)]
    qb = [P.sb("qb%d" % i, [128, 512], BF16) for i in range(2)]
    sq = P.sb("sq", [128, 512], BF16)
    qn = [P.sb("qn%d" % i, [128, 512], F32) for i in range(2)]
    t1 = [P.sb("t1%d" % i, [128, 512], F32) for i in range(2)]
    t2 = [P.sb("t2%d" % i, [128, 512], F32) for i in range(2)]
    ps3 = P.psum[0:3]
    Sq = [(P.psum[3], 0), (P.psum[4], 0), (P.psum[7], 0)]
    Sk = [(P.psum[5], 0), (P.psum[6], 0), (P.psum[7], 64)]
    m_phase = P.mark()
    hT = P.sb("hT", [128, KC, T], BF16)
    xbufs = [P.sb("xb%d" % i, [128, T], F32) for i in range(2)]
    modulate_load(P, xT_d, sc1p, C["sh"], hT, xbufs)
    widx = 0
    for jj in range(12):
        isq = jj < 8
        dst, jdx, S, ones_, nch = (cq32, jj, Sq, onesq, 8) if isq else (ckv32, jj - 8, Sk, onesk, 4)
        gemm_fm(P, hT, KC, w_d, jj * 128, 128, wbufs, ps3, widx)
        widx += 1
        for ti, (t0, tn) in enumerate(TT):
            P.op("act", lambda e, ti=ti, t0=t0, tn=tn, dst=dst, jdx=jdx: e.activation(
                out=dst[:, jdx, t0:t0 + tn], in_=ps3[ti][:, 0:tn], func=AF.Copy), reads=[ps3[ti]], writes=[dst])
            P.op("act", lambda e, ti=ti, tn=tn: e.activation(out=sq[:, 0:tn], in_=ps3[ti][:, 0:tn], func=AF.Square),
                 reads=[ps3[ti]], writes=[sq])
            st, scol = S[ti]
            P.op("pe", lambda e, st=st, scol=scol, tn=tn, ones_=ones_, jdx=jdx, nch=nch: e.matmul(
                st[:, scol:scol + tn], lhsT=ones_[:], rhs=sq[:, 0:tn], start=(jdx == 0), stop=(jdx == nch - 1)),
                reads=[ones_, sq], writes=[st])
    for S, rs_ in ((Sq, rsq), (Sk, rskv)):
        for ti, (t0, tn) in enumerate(TT):
            st, scol = S[ti]
            P.op("act", lambda e, st=st, scol=scol, t0=t0, tn=tn, rs_=rs_: e.activation(
                out=rs_[:, t0:t0 + tn], in_=st[:, scol:scol + tn], func=AF.Sqrt, bias=epsr[:, 0:1], scale=1.0),
                reads=[st, epsr], writes=[rs_])
        P.op("dve", lambda e, rs_=rs_: e.reciprocal(out=rs_[:], in_=rs_[:]), reads=[rs_], writes=[rs_])
    ps_r = P.psum[3]
    gemm_fm(P, hT, KC, w_d, 1536, 64, wbufs, ps3, widx)
    widx += 1
    ob = obufs[0]
    for ti, (t0, tn) in enumerate(TT):
        rope_tile(P, ps3[ti], 64, t0, tn, ti % 2, qn, qb, t1, t2, rot, C["cosT"], C["sinT"], ps_r, ob)
    P.dma("sp", KR_d.ap(), ob[0:64, :], ob, reads=[ob])
    P.fence()
    P.release(m_phase)
    cqn = P.sb("cqn", [128, 8, T], BF16)
    ckvn = P.sb("ckvn", [128, 4, T], BF16)
    for j in range(8):
        P.op("dve", lambda e, j=j: e.scalar_tensor_tensor(out=cqn[:, j, :], in0=cq32[:, j, :], scalar=C["gqn"][:, j:j + 1],
                                                          in1=rsq[:], op0=ALU.mult, op1=ALU.mult),
             reads=[cq32, C["gqn"], rsq], writes=[cqn])
    for j in range(4):
        P.op("dve", lambda e, j=j: e.scalar_tensor_tensor(out=ckvn[:, j, :], in0=ckv32[:, j, :], scalar=C["gkvn"][:, j:j + 1],
                                                          in1=rskv[:], op0=ALU.mult, op1=ALU.mult),
             reads=[ckv32, C["gkvn"], rskv], writes=[ckvn])
    import os
    nh = int(os.environ.get("K_NH", str(NH)))
    oi = 0
    for h in range(nh):
        for (src, nk, wdr, col0, ncol, kind, od) in (
                (cqn, 8, wuq_d, h * 192, 128, "plain", QN_d), (cqn, 8, wuq_d, h * 192 + 128, 64, "rope", QR_d),
                (ckvn, 4, wukv_d, h * 256, 128, "plain", KN_d), (ckvn, 4, wukv_d, h * 256 + 128, 128, "plain", VT_d)):
            gemm_fm(P, src, nk, wdr, col0, ncol, wbufs, ps3, widx)
            widx += 1
            ob = obufs[oi % 3]
            oi += 1
            for ti, (t0, tn) in enumerate(TT):
                if kind == "plain":
                    P.op("act", lambda e, ti=ti, t0=t0, tn=tn, ob=ob: e.activation(
                        out=ob[:, t0:t0 + tn], in_=ps3[ti][:, 0:tn], func=AF.Copy), reads=[ps3[ti]], writes=[ob])
                else:
                    rope_tile(P, ps3[ti], 64, t0, tn, ti % 2, qn, qb, t1, t2, rot, C["cosT"], C["sinT"], ps_r, ob)
            P.dma("sp", od.ap()[h], ob[0:ncol, :], ob, reads=[ob])
    return P.finish()


def build_odd_b1():
    P = Prog()
    QN_d = P.dram("QN", [NH, 128, T], BF16, "ExternalInput")
    QR_d = P.dram("QR", [NH, 64, T], BF16, "ExternalInput")
    KN_d = P.dram("KNall", [NH, 128, NKEY], BF16, "ExternalInput")
    KR_d = P.dram("KRall", [64, NKEY], BF16, "ExternalInput")
    V_d = P.dram("Vall", [NH, 128, NKB, 128], BF16, "ExternalInput")
    AT_d = P.dram("attnT", [NH, 128, T], BF16, "ExternalOutput")
    scale = 1.0 / math.sqrt(192.0)
    onesb = P.sb("onesb", [128, 128], BF16)
    P.op("dve", lambda e: e.memset(onesb[:], 1.0), writes=[onesb])
    onesf = P.sb("onesf", [128, 128], F32)
    P.op("dve", lambda e: e.memset(onesf[:], 1.0), writes=[onesf])
    accL = P.sb("accL", [128, 2, 512], F32)
    KR = P.sb("KR", [64, NKEY], BF16)
    P.dma("sp", KR[:], KR_d.ap(), KR, writes=[KR])
    Kb = [P.sb("Kb%d" % i, [128, NKEY], BF16) for i in range(3)]
    Vb = [P.sb("Vb%d" % i, [128, NKB, 128], BF16) for i in range(3)]
    Qb = [P.sb("Qb%d" % i, [128, T], BF16) for i in range(3)]
    Qr = [P.sb("Qr%d" % i, [64, T], BF16) for i in range(3)]
    Pb = [P.sb("Pb%d" % i, [128, 2, 512], BF16) for i in range(2)]
    A1 = [P.sb("A1%d" % i, [128, T], F32) for i in range(2)]
    rl = P.sb("rl", [128, 512], F32)
    of = P.sb("of", [128, 512], F32)
    obs = [P.sb("obs%d" % i, [128, T], BF16) for i in range(3)]
    psS = [(P.pp[0], P.psum[0], P.psum[1]), (P.pp[1], P.psum[2], P.psum[3])]
    psO, psL = P.psum[4:6], P.psum[6]
    import os
    nh = int(os.environ.get("K_NH", str(NH)))
    cnt = 0
    for h in range(nh):
        i3 = h % 3
        P.dma("sp", Qb[i3][:], QN_d.ap()[h], Qb[i3], writes=[Qb[i3]])
        P.dma("sp", Qr[i3][:], QR_d.ap()[h], Qr[i3], writes=[Qr[i3]])
        P.dma("sp", Kb[i3][:], KN_d.ap()[h], Kb[i3], writes=[Kb[i3]])
        P.dma("sp", Vb[i3][:], V_d.ap()[h], Vb[i3], writes=[Vb[i3]])
        cnt = attn_unit(P, Qb[i3], Kb[i3], [Vb[i3]], [A1[h % 2]], Pb, psS, psO, psL, onesb, scale, rl, of, cnt,
                        Qt2=Qr[i3], Kt2=KR, accL=accL, onesf=onesf)
        ob = obs[i3]
        P.op("act", lambda e, h=h, ob=ob: e.activation(out=ob[:], in_=A1[h % 2][:], func=AF.Copy),
             reads=[A1[h % 2]], writes=[ob])
        P.dma("sp", AT_d.ap()[h], ob[:], ob, reads=[ob])
    return P.finish()


_CACHE = {}


def _get(name, builder, *args):
    key = (name,) + tuple(args)
    if key not in _CACHE:
        _CACHE[key] = builder(*args)
    return _CACHE[key]


def _f32(a):
    return np.ascontiguousarray(np.asarray(a, dtype=np.float32))


def _mods_all(c, c_ctx, w_ada, b_ada):
    nc = _get("mods", build_mods)
    cT = np.ascontiguousarray(np.stack([c[0], c_ctx], -1).reshape(KC, 128, 2).transpose(1, 0, 2))
    in_maps = []
    for j in range(NCORES):
        wad = np.ascontiguousarray(w_ada[:, :, j * MCOLS:(j + 1) * MCOLS])
        bad = np.ascontiguousarray(np.broadcast_to(
            b_ada[:, j * MCOLS:(j + 1) * MCOLS].reshape(1, -1), (2, DEPTH * MCOLS)))
        in_maps.append({"cT": cT, "wad": wad, "bad": bad})
    res = run(nc, in_maps)
    mo = np.concatenate([r["mo"].reshape(2, DEPTH, MCOLS) for r in res], axis=2)
    return mo[0].reshape(DEPTH, 6, D), mo[1].reshape(DEPTH, 6, D)


def _gather_keys(parts):
    lat = np.concatenate([p[:, :, 0:LAT] for p in parts], axis=2)
    ctx = np.concatenate([p[:, :, LAT:T] for p in parts], axis=2)
    return np.ascontiguousarray(np.concatenate([lat, ctx], axis=2))


def _values_layout(vt_all):
    C = vt_all.shape[0]
    v = vt_all.reshape(C, 128, NKB, 128)
    return np.ascontiguousarray(v.transpose(0, 3, 2, 1))


def _even_attention(layer, xT_shards, ml, mc, w_in, gq, gk, lam_v, subg):
    lambda_init = 0.8 - 0.6 * math.exp(-0.3 * layer)
    nca = _get("even_a", build_even_a)
    rotm = rot_matrix(128)
    sc, sh = fm2(ml[1], mc[1]), fm2(ml[0], mc[0])
    in_maps = []
    for j in range(NCORES):
        cos, sin = rope_tables(j, 128)
        in_maps.append({"xT": xT_shards[j], "w_in": w_in, "sc": sc, "sh": sh,
                        "cosT": cos, "sinT": sin, "gq": np.ascontiguousarray(gq.reshape(128, 1)),
                        "gk": np.ascontiguousarray(gk.reshape(128, 1)), "rotm": rotm})
    ra = run(nca, in_maps)
    kt_all = _gather_keys([r["KT"] for r in ra])
    v_all = _values_layout(_gather_keys([r["VT"] for r in ra]))
    ncb = _get("even_b1", build_even_b1, lambda_init)
    lamv = np.ascontiguousarray(lam_v.T)
    sg = np.ascontiguousarray(subg.reshape(2, 128).T)
    in_maps = [{"QT": ra[j]["QT"], "KTall": kt_all, "Vall": v_all, "lamv": lamv, "subg": sg}
               for j in range(NCORES)]
    rb = run(ncb, in_maps)
    return [r["attnT"] for r in rb]


def _odd_attention(xT_shards, ml, mc, w_in, gqn, gkvn, w_uq, w_ukv):
    nca = _get("odd_a", build_odd_a)
    rotm = rot_matrix(64)
    sc, sh = fm2(ml[1], mc[1]), fm2(ml[0], mc[0])
    gq = np.ascontiguousarray(gqn.reshape(8, 128).T)
    gk = np.ascontiguousarray(gkvn.reshape(4, 128).T)
    in_maps = []
    for j in range(NCORES):
        cos, sin = rope_tables(j, 64)
        in_maps.append({"xT": xT_shards[j], "w_in": w_in, "w_uq": w_uq, "w_ukv": w_ukv, "sc": sc, "sh": sh,
                        "cosT": cos, "sinT": sin, "gqn": gq, "gkvn": gk, "rotm": rotm})
    ra = run(nca, in_maps)
    kn_all = _gather_keys([r["KN"] for r in ra])
    v_all = _values_layout(_gather_keys([r["VT"] for r in ra]))
    kr_all = np.ascontiguousarray(_gather_keys([r["KR"][None] for r in ra])[0])
    ncb = _get("odd_b1", build_odd_b1)
    in_maps = [{"QN": ra[j]["QN"], "QR": ra[j]["QR"], "KNall": kn_all, "KRall": kr_all, "Vall": v_all}
               for j in range(NCORES)]
    rb = run(ncb, in_maps)
    return [r["attnT"] for r in rb]


def _post(attnT, xT_shards, ml, mc, w_out, ln1g, ln1b, ln2g, ln2b, w_group, b_group, w_router, b_router,
          w_gate, w_up, w_down):
    nc = _get("post", build_post)
    common = {
        "w_out": w_out, "w_gate": w_gate, "w_up": w_up,
        "w_down": np.ascontiguousarray(w_down.reshape(NEXP * DEXP, D)),
        "wgr": np.ascontiguousarray(np.concatenate([w_group, w_router], axis=1)),
        "bgr": np.ascontiguousarray(np.broadcast_to(np.concatenate([b_group, b_router])[None, :], (128, 20))),
        "g1": fm2(ml[2], mc[2]), "sc2": fm2(ml[4], mc[4]), "sh2": fm2(ml[3], mc[3]), "g2": fm2(ml[5], mc[5]),
        "ln1g": fm(ln1g), "ln1b": fm(ln1b), "ln2g": fm(ln2g), "ln2b": fm(ln2b),
        "identf": np.eye(128, dtype=np.float32), "selm": sel_matrix()}
    in_maps = [dict(common, attnT=attnT[j], xT=xT_shards[j]) for j in range(NCORES)]
    res = run(nc, in_maps)
    return [r["xT_new"] for r in res]


def kernel(x, c, ctx, c_ctx, w_ada, b_ada, ln1_g, ln1_b, ln2_g, ln2_b,
           ev_w_in, ev_w_out, diff_lambda, diff_subln_g, gqa_q_norm_g, gqa_k_norm_g,
           od_w_in, mla_q_norm_g, mla_kv_norm_g, mla_w_uq, mla_w_ukv, od_w_out,
           moe_w_group, moe_b_group, moe_w_router, moe_b_router, moe_w_gate, moe_w_up, moe_w_down):
    x = _f32(x)
    ctx = _f32(ctx)
    mods_l, mods_c = _mods_all(_f32(c), _f32(c_ctx), _f32(w_ada), _f32(b_ada))
    xT = [shard_tokens_T(x[0], ctx[0], j) for j in range(NCORES)]
    for layer in range(DEPTH):
        ml, mc = mods_l[layer], mods_c[layer]
        i = layer // 2
        if layer % 2 == 0:
            attnT = _even_attention(layer, xT, ml, mc, _f32(ev_w_in[i]), _f32(gqa_q_norm_g[i]),
                                    _f32(gqa_k_norm_g[i]), _f32(diff_lambda[i]), _f32(diff_subln_g[i]))
            w_out = _f32(ev_w_out[i])
        else:
            attnT = _odd_attention(xT, ml, mc, _f32(od_w_in[i]), _f32(mla_q_norm_g[i]), _f32(mla_kv_norm_g[i]),
                                   _f32(mla_w_uq[i]), _f32(mla_w_ukv[i]))
            w_out = _f32(od_w_out[i])
        xT = _post(attnT, xT, ml, mc, w_out, _f32(ln1_g[layer]), _f32(ln1_b[layer]), _f32(ln2_g[layer]),
                   _f32(ln2_b[layer]), _f32(moe_w_group[layer]), _f32(moe_b_group[layer]),
                   _f32(moe_w_router[layer]), _f32(moe_b_router[layer]), _f32(moe_w_gate[layer]),
                   _f32(moe_w_up[layer]), _f32(moe_w_down[layer]))
    out = np.concatenate([xT[j][:, 0:LAT].T for j in range(NCORES)], axis=0)
    return np.ascontiguousarray(out.reshape(1, SEQ, D).astype(np.float32))
```

```python
import math
import numpy as np
import ml_dtypes
import concourse.bass as bass
import concourse.mybir as mybir
from concourse.bass_utils import run_bass_kernel_spmd

F32 = mybir.dt.float32
BF16 = mybir.dt.bfloat16
AF = mybir.ActivationFunctionType
ALU = mybir.AluOpType

NCORES = 8
D = 4096
KC = 32
SEQ = 8192
CTX = 256
LAT = SEQ // NCORES
CT = CTX // NCORES
T = LAT + CT
TT = [(0, 512), (512, 512), (1024, 32)]
NKB = (SEQ + CTX) // 128
DEPTH = 4
ALPHA = (2.0 * DEPTH) ** 0.25
LN_EPS = 1e-5
RMS_EPS = 1e-6
GRID_W = 64


class Buf:
    __slots__ = ("w", "r", "dsem", "dcnt", "name")

    def __init__(self, name=""):
        self.w = None
        self.r = []
        self.dsem = None
        self.dcnt = 0
        self.name = name


class Tile:
    def __init__(self, handle, name):
        self.t = handle
        self.b = Buf(name)

    def __getitem__(self, idx):
        return self.t[idx]


class Prog:
    ENG = ("pe", "act", "dve", "pool", "sp")
    COMPUTE = ("pe", "act", "dve", "pool")

    def __init__(self):
        self.nc = bass.Bass("TRN2", target_bir_lowering=False)
        self.q = {e: [] for e in self.ENG}
        self.sems = []
        self.sem = {}
        self.cnt = {}
        for e in self.COMPUTE:
            self.sem[e] = self._newsem("c_" + e)
            self.cnt[e] = 0
        self.seen = {e: {} for e in self.ENG}
        self.sb_off = 16640
        self.sb_max = 229000
        self.nid = 0
        self.dma_bufs = []
        self.psum = []
        for i in range(8):
            h = self.nc.alloc_psum_tensor("ps%d" % i, [128, 512], F32)
            self.psum.append(Tile(h, "ps%d" % i))

    def _newsem(self, name):
        h = self.nc.alloc_semaphore(name)
        self.sems.append(h)
        return (len(self.sems) - 1, h)

    def dram(self, name, shape, dtype, kind):
        return self.nc.dram_tensor(name, list(shape), dtype, kind=kind)

    def sb(self, name, shape, dtype):
        esz = 4 if dtype == F32 else 2
        per = esz
        for s in shape[1:]:
            per *= s
        per = (per + 63) // 64 * 64
        off = self.sb_off
        assert off + per <= self.sb_max, ("SBUF overflow", name, off, per)
        self.nid += 1
        h = self.nc.alloc_sbuf_tensor_at("%s_%d" % (name, self.nid), list(shape), dtype, offset=off)
        self.sb_off = off + per
        return Tile(h, name)

    def mark(self):
        return self.sb_off

    def release(self, m):
        self.sb_off = m

    def _collect(self, eng, reads, writes):
        need = {}

        def add(tok):
            if tok is None:
                return
            key, h, v, src = tok
            if src == "pe" and eng == "pe":
                return
            if self.seen[eng].get(key, 0) >= v:
                return
            if key not in need or need[key][1] < v:
                need[key] = (h, v)

        for b in reads:
            add(b.w)
        for b in writes:
            add(b.w)
            for t in b.r:
                add(t)
        out = []
        for key, (h, v) in need.items():
            self.seen[eng][key] = v
            out.append((h, v))
        return out

    def _commit(self, tok, reads, writes):
        for b in reads:
            b.r.append(tok)
        for b in writes:
            b.w = tok
            b.r = []

    def op(self, eng, fn, reads=(), writes=()):
        reads = [x.b if isinstance(x, Tile) else x for x in reads]
        writes = [x.b if isinstance(x, Tile) else x for x in writes]
        waits = self._collect(eng, reads, writes)
        self.cnt[eng] += 1
        key, h = self.sem[eng]
        tok = (key, h, self.cnt[eng], eng)
        self.q[eng].append((waits, fn, h, 1))
        self._commit(tok, reads, writes)

    def dma(self, queue, out_ap, in_ap, slot, reads=(), writes=()):
        reads = [x.b if isinstance(x, Tile) else x for x in reads]
        writes = [x.b if isinstance(x, Tile) else x for x in writes]
        slot = slot.b if isinstance(slot, Tile) else slot
        if slot.dsem is None:
            slot.dsem = self._newsem("d_%s_%d" % (slot.name, len(self.sems)))
            self.dma_bufs.append(slot)
        waits = self._collect(queue, reads, writes)
        slot.dcnt += 1
        key, h = slot.dsem
        tok = (key, h, 16 * slot.dcnt, "dma")
        self.q[queue].append((waits, lambda e: e.dma_start(out=out_ap, in_=in_ap), h, 16))
        self._commit(tok, reads, writes)

    def fence(self):
        cur = []
        for e in self.COMPUTE:
            key, h = self.sem[e]
            if self.cnt[e] > 0:
                cur.append((key, h, self.cnt[e]))
        for b in self.dma_bufs:
            key, h = b.dsem
            cur.append((key, h, 16 * b.dcnt))
        for e in self.ENG:
            waits = []
            for key, h, v in cur:
                if self.seen[e].get(key, 0) < v:
                    self.seen[e][key] = v
                    waits.append((h, v))
            if waits:
                self.q[e].append((waits, None, None, 0))

    def finish(self):
        self.fence()
        nc = self.nc
        q = self.q

        def replay(e, items):
            for waits, fn, h, inc in items:
                for (sh, v) in waits:
                    e.wait_ge(sh, v)
                if fn is not None:
                    ins = fn(e)
                    ins.then_inc(h, inc)

        with nc.Block() as block:
            @block.tensor
            def _(e):
                replay(e, q["pe"])

            @block.scalar
            def _(e):
                replay(e, q["act"])

            @block.vector
            def _(e):
                replay(e, q["dve"])

            @block.gpsimd
            def _(e):
                replay(e, q["pool"])

            @block.sync
            def _(e):
                replay(e, q["sp"])
        return nc


def run(prog_nc, in_maps):
    res = run_bass_kernel_spmd(prog_nc, in_maps, core_ids=list(range(NCORES)))
    return res.results


MCOLS = 6 * D // NCORES


def build_mods():
    P = Prog()
    nc = P.nc
    cT = P.dram("cT", [128, KC, 2], F32, "ExternalInput")
    wad = P.dram("wad", [DEPTH, D, MCOLS], F32, "ExternalInput")
    bad = P.dram("bad", [2, DEPTH * MCOLS], F32, "ExternalInput")
    mo = P.dram("mo", [2, DEPTH * MCOLS], F32, "ExternalOutput")
    c_sb = P.sb("c_sb", [128, KC, 2], F32)
    s_sb = P.sb("s_sb", [128, KC, 2], F32)
    b_sb = P.sb("b_sb", [2, DEPTH * MCOLS], F32)
    o_sb = P.sb("o_sb", [2, DEPTH * MCOLS], F32)
    MB = 256
    wts = [P.sb("wt%d" % i, [128, KC, MB], F32) for i in range(2)]
    P.dma("sp", c_sb[:], cT.ap(), c_sb, writes=[c_sb])
    P.dma("sp", b_sb[:], bad.ap(), b_sb, writes=[b_sb])
    P.op("act", lambda e: e.activation(out=s_sb[:], in_=c_sb[:], func=AF.Silu),
         reads=[c_sb], writes=[s_sb])
    nblk = DEPTH * MCOLS // MB
    for blk in range(nblk):
        l, cb = divmod(blk, MCOLS // MB)
        wt = wts[blk % 2]
        src = wad.ap()[l, :, cb * MB:(cb + 1) * MB].rearrange("(kc p) n -> p kc n", p=128)
        P.dma("sp", wt[:], src, wt, writes=[wt])
        ps = P.psum[blk % 2]

        def mm(e, wt=wt, ps=ps):
            ins = None
            for kc in range(KC):
                ins = e.matmul(ps[0:2, 0:MB], lhsT=s_sb[:, kc, :], rhs=wt[:, kc, :],
                               start=(kc == 0), stop=(kc == KC - 1))
            return ins
        P.op("pe", mm, reads=[s_sb, wt], writes=[ps])
        P.op("dve", lambda e, ps=ps, blk=blk: e.tensor_tensor(
            out=o_sb[:, blk * MB:(blk + 1) * MB], in0=ps[0:2, 0:MB],
            in1=b_sb[:, blk * MB:(blk + 1) * MB], op=ALU.add),
            reads=[ps, b_sb], writes=[o_sb])
    P.dma("sp", mo.ap(), o_sb[:], o_sb, reads=[o_sb])
    return P.finish()


def load_consts(P, names_shapes):
    out = {}
    for name, shape in names_shapes:
        d = P.dram(name, shape, F32, "ExternalInput")
        t = P.sb(name + "_sb", shape, F32)
        P.dma("sp", t[:], d.ap(), t, writes=[t])
        out[name] = t
    return out


def modulate_load(P, xT_d, sc1p, sh, hT, xbufs):
    for kc in range(KC):
        xb = xbufs[kc % len(xbufs)]
        P.dma("sp", xb[:], xT_d.ap()[kc * 128:(kc + 1) * 128, :], xb, writes=[xb])

        def f(e, kc=kc, xb=xb):
            e.activation(out=hT[:, kc, 0:LAT], in_=xb[:, 0:LAT], func=AF.Identity,
                         bias=sh[:, kc, 0:1], scale=sc1p[:, kc, 0:1])
            return e.activation(out=hT[:, kc, LAT:T], in_=xb[:, LAT:T], func=AF.Identity,
                                bias=sh[:, kc, 1:2], scale=sc1p[:, kc, 1:2])
        P.op("act", f, reads=[xb, sc1p, sh], writes=[hT])


def gemm_fm(P, hT, nk, w_d, col0, ncol, wbufs, ps3, widx):
    wt = wbufs[widx % len(wbufs)]
    w_ap = w_d if isinstance(w_d, bass.AP) else w_d.ap()
    src = w_ap[:, col0:col0 + ncol].rearrange("(kc p) n -> p kc n", p=128)
    P.dma("pool", wt[:, 0:nk, 0:ncol], src, wt, writes=[wt])

    def mm(e):
        ins = None
        for kc in range(nk):
            for ti, (t0, tn) in enumerate(TT):
                ins = e.matmul(ps3[ti][0:ncol, 0:tn], lhsT=wt[:, kc, 0:ncol], rhs=hT[:, kc, t0:t0 + tn],
                               start=(kc == 0), stop=(kc == nk - 1))
        return ins
    P.op("pe", mm, reads=[wt, hT], writes=list(ps3))


def make_rot(P, rot_d_name="rotm", n=128):
    d = P.dram(rot_d_name, [n, n], F32, "ExternalInput")
    t32 = P.sb(rot_d_name + "32", [n, n], F32)
    tb = P.sb(rot_d_name + "b", [n, n], BF16)
    P.dma("sp", t32[:], d.ap(), t32, writes=[t32])
    P.op("dve", lambda e: e.tensor_copy(out=tb[:], in_=t32[:]), reads=[t32], writes=[tb])
    return tb


def rot_matrix(n):
    half = n // 2
    m = np.zeros((n, n), np.float32)
    for j in range(half):
        m[j + half, j] = -1.0
        m[j, j + half] = 1.0
    return m


EV_IN = 9216


def build_even_a():
    P = Prog()
    xT_d = P.dram("xT", [D, T], F32, "ExternalInput")
    w_d = P.dram("w_in", [D, EV_IN], F32, "ExternalInput")
    QT_d = P.dram("QT", [32, 128, T], BF16, "ExternalOutput")
    KT_d = P.dram("KT", [20, 128, T], BF16, "ExternalOutput")
    VT_d = P.dram("VT", [20, 128, T], BF16, "ExternalOutput")
    C = load_consts(P, [("sc", [128, KC, 2]), ("sh", [128, KC, 2]), ("cosT", [128, T]),
                        ("sinT", [128, T]), ("gq", [128, 1]), ("gk", [128, 1])])
    rot = make_rot(P)
    ones = P.sb("ones", [128, 128], BF16)
    P.op("dve", lambda e: e.memset(ones[:], 1.0 / 128.0), writes=[ones])
    epsr = P.sb("epsr", [128, 1], F32)
    P.op("dve", lambda e: e.memset(epsr[:], RMS_EPS), writes=[epsr])
    sc1p = P.sb("sc1p", [128, KC, 2], F32)
    P.op("dve", lambda e: e.tensor_scalar(out=sc1p[:], in0=C["sc"][:], scalar1=1.0, scalar2=None,
                                          op0=ALU.add), reads=[C["sc"]], writes=[sc1p])
    hT = P.sb("hT", [128, KC, T], BF16)
    xbufs = [P.sb("xb%d" % i, [128, T], F32) for i in range(3)]
    import os
    if os.environ.get("K_STAGE") == "0":
        return P.finish()
    modulate_load(P, xT_d, sc1p, C["sh"], hT, xbufs)
    if os.environ.get("K_STAGE") == "1":
        return P.finish()
    wbufs = [P.sb("wb%d" % i, [128, KC, 128], BF16) for i in range(3)]
    obufs = [P.sb("ob%d" % i, [128, T], BF16) for i in range(3)]
    qb = [P.sb("qb%d" % i, [128, 512], BF16) for i in range(2)]
    sq = [P.sb("sq%d" % i, [128, 512], BF16) for i in range(2)]
    rs = [P.sb("rs%d" % i, [128, 512], F32) for i in range(2)]
    qn = [P.sb("qn%d" % i, [128, 512], F32) for i in range(2)]
    t1 = [P.sb("t1%d" % i, [128, 512], F32) for i in range(2)]
    t2 = [P.sb("t2%d" % i, [128, 512], F32) for i in range(2)]
    cosT, sinT = C["cosT"], C["sinT"]
    psA = [P.psum[0:3], P.psum[3:6]]
    ps_r, ps_s = P.psum[6], P.psum[7]
    plan = []
    for i in range(16):
        plan.append(("rope", QT_d, i, None))
    for i in range(16):
        plan.append(("rope", KT_d, i, None))
    for i in range(16):
        plan.append(("plain", VT_d, i, None))
    for i in range(16):
        plan.append(("rms", QT_d, 16 + i, C["gq"]))
    for i in range(4):
        plan.append(("rms", KT_d, 16 + i, C["gk"]))
    for i in range(4):
        plan.append(("plain", VT_d, 16 + i, None))
    cnt = 0
    import os
    plan = plan[:int(os.environ.get('K_PLAN', '999'))]
    for oc, (kind, od, oi, g) in enumerate(plan):
        ps3 = psA[oc % 2]
        gemm_fm(P, hT, KC, w_d, oc * 128, 128, wbufs, ps3, oc)
        if os.environ.get("K_STAGE") == "2":
            return P.finish()
        ob = obufs[oc % 3]
        for ti, (t0, tn) in enumerate(TT):
            ps = ps3[ti]
            j = cnt % 2
            cnt += 1
            if kind == "plain":
                P.op("act", lambda e, ps=ps, t0=t0, tn=tn, ob=ob: e.activation(
                    out=ob[:, t0:t0 + tn], in_=ps[:, 0:tn], func=AF.Copy), reads=[ps], writes=[ob])
                continue
            if kind == "rms":
                P.op("act", lambda e, ps=ps, tn=tn, j=j: e.activation(
                    out=sq[j][:, 0:tn], in_=ps[:, 0:tn], func=AF.Square), reads=[ps], writes=[sq[j]])
                P.op("pe", lambda e, tn=tn, j=j: e.matmul(ps_s[:, 0:tn], lhsT=ones[:], rhs=sq[j][:, 0:tn],
                                                          start=True, stop=True),
                     reads=[ones, sq[j]], writes=[ps_s])
                P.op("act", lambda e, tn=tn, j=j: e.activation(
                    out=rs[j][:, 0:tn], in_=ps_s[:, 0:tn], func=AF.Sqrt, bias=epsr[:, 0:1], scale=1.0),
                    reads=[ps_s, epsr], writes=[rs[j]])
                P.op("dve", lambda e, tn=tn, j=j: e.reciprocal(out=rs[j][:, 0:tn], in_=rs[j][:, 0:tn]),
                     reads=[rs[j]], writes=[rs[j]])
                P.op("act", lambda e, ps=ps, tn=tn, j=j: e.activation(
                    out=qn[j][:, 0:tn], in_=ps[:, 0:tn], func=AF.Copy), reads=[ps], writes=[qn[j]])
                P.op("dve", lambda e, tn=tn, j=j, g=g: e.scalar_tensor_tensor(
                    out=qn[j][:, 0:tn], in0=qn[j][:, 0:tn], scalar=g[:, 0:1], in1=rs[j][:, 0:tn],
                    op0=ALU.mult, op1=ALU.mult), reads=[qn[j], rs[j], g], writes=[qn[j]])
                src_t, src_ap = qn[j], qn[j]
            else:
                P.op("act", lambda e, ps=ps, tn=tn, j=j: e.activation(
                    out=qn[j][:, 0:tn], in_=ps[:, 0:tn], func=AF.Copy), reads=[ps], writes=[qn[j]])
                src_t = qn[j]
            P.op("act", lambda e, s=src_t, tn=tn, j=j: e.activation(
                out=qb[j][:, 0:tn], in_=s[:, 0:tn], func=AF.Copy), reads=[src_t], writes=[qb[j]])
            P.op("pe", lambda e, tn=tn, j=j: e.matmul(ps_r[:, 0:tn], lhsT=rot[:], rhs=qb[j][:, 0:tn],
                                                      start=True, stop=True),
                 reads=[rot, qb[j]], writes=[ps_r])
            if os.environ.get("K_STAGE") == "3":
                return P.finish()
            if os.environ.get("K_SKIPDVE") == "1":
                continue
            P.op("dve", lambda e, s=src_t, t0=t0, tn=tn, j=j: e.tensor_tensor(
                out=t1[j][:, 0:tn], in0=(sinT[:, t0:t0 + tn] if os.environ.get("K_NOPS") else s[:, 0:tn]), in1=cosT[:, t0:t0 + tn], op=ALU.mult),
                reads=[src_t, cosT], writes=[t1[j]])
            if os.environ.get("K_DVE") == "1":
                continue
            P.op("act", lambda e, tn=tn, j=j: e.activation(
                out=t2[j][:, 0:tn], in_=ps_r[:, 0:tn], func=AF.Copy), reads=[ps_r], writes=[t2[j]])
            P.op("dve", lambda e, t0=t0, tn=tn, j=j: e.tensor_tensor(
                out=t2[j][:, 0:tn], in0=t2[j][:, 0:tn], in1=sinT[:, t0:t0 + tn], op=ALU.mult),
                reads=[t2[j], sinT], writes=[t2[j]])
            if os.environ.get("K_DVE") == "2":
                continue
            P.op("dve", lambda e, t0=t0, tn=tn, j=j, ob=ob: e.tensor_tensor(
                out=ob[:, t0:t0 + tn], in0=t1[j][:, 0:tn], in1=t2[j][:, 0:tn], op=ALU.add),
                reads=[t1[j], t2[j]], writes=[ob])
        if os.environ.get("K_STAGE") == "4":
            return P.finish()
        P.dma("sp", od.ap()[oi], ob[:], ob, reads=[ob])
    return P.finish()


def rope_tables(core, rot_dim):
    idx = core * LAT + np.arange(LAT)
    row = (idx // GRID_W).astype(np.float32)
    col = (idx % GRID_W).astype(np.float32)
    quarter = rot_dim // 4
    inv = (10000.0 ** (-np.arange(quarter, dtype=np.float32) / quarter)).astype(np.float32)
    ang = np.concatenate([row[:, None] * inv, col[:, None] * inv], axis=-1)
    cos = np.ones((rot_dim, T), np.float32)
    sin = np.zeros((rot_dim, T), np.float32)
    c, s = np.cos(ang).T.astype(np.float32), np.sin(ang).T.astype(np.float32)
    h = rot_dim // 2
    cos[0:h, 0:LAT] = c
    cos[h:, 0:LAT] = c
    sin[0:h, 0:LAT] = s
    sin[h:, 0:LAT] = s
    return cos, sin


def fm(vec):
    return np.ascontiguousarray(vec.reshape(-1, 128).T)


def fm2(v_lat, v_ctx):
    return np.ascontiguousarray(np.stack([fm(v_lat), fm(v_ctx)], axis=-1))


def shard_tokens_T(x_lat, x_ctx, core):
    a = x_lat[core * LAT:(core + 1) * LAT]
    b = x_ctx[core * CT:(core + 1) * CT]
    return np.ascontiguousarray(np.concatenate([a, b], axis=0).T)


NKEY = SEQ + CTX
QTILES = [(0, 512, list(range(NKB))), (512, 512, list(range(NKB))), (LAT, CT, [NKB - 2, NKB - 1])]


def attn_unit(P, Qt, Kt, Vts, outs, Pb, psS, psO, psL, onesb, scale, rl, of, cnt0, Qt2=None, Kt2=None):
    cnt = cnt0
    NS = len(psS)
    LA = NS - 1
    for (t0, tn, kbs) in QTILES:
        n = len(kbs)

        def rec_qk(i, t0=t0, tn=tn, kbs=kbs, base=cnt):
            kb = kbs[i]
            S = psS[(base + i) % NS]
            Pt = Pb[(base + i) % NS]
            if Qt2 is None:
                P.op("pe", lambda e: e.matmul(
                    S[:, 0:tn], lhsT=Kt[:, kb * 128:(kb + 1) * 128], rhs=Qt[:, t0:t0 + tn], start=True, stop=True),
                    reads=[Kt, Qt], writes=[S])
            else:
                def qk(e):
                    e.matmul(S[:, 0:tn], lhsT=Kt[:, kb * 128:(kb + 1) * 128], rhs=Qt[:, t0:t0 + tn],
                             start=True, stop=False)
                    return e.matmul(S[:, 0:tn], lhsT=Kt2[0:64, kb * 128:(kb + 1) * 128], rhs=Qt2[0:64, t0:t0 + tn],
                                    start=False, stop=True)
                P.op("pe", qk, reads=[Kt, Qt, Kt2, Qt2], writes=[S])
            P.op("act", lambda e: e.activation(out=Pt[:, 0:tn], in_=S[:, 0:tn], func=AF.Exp, scale=scale),
                 reads=[S], writes=[Pt])

        def rec_pv(i, tn=tn, kbs=kbs, base=cnt, n=n):
            kb = kbs[i]
            Pt = Pb[(base + i) % NS]
            first, last = (i == 0), (i == n - 1)

            def pv(e):
                for vi, Vt in enumerate(Vts):
                    e.matmul(psO[vi][:, 0:tn], lhsT=Vt[:, kb, :], rhs=Pt[:, 0:tn], start=first, stop=last)
                return e.matmul(psL[:, 0:tn], lhsT=onesb[:], rhs=Pt[:, 0:tn], start=first, stop=last)
            P.op("pe", pv, reads=list(Vts) + [Pt, onesb], writes=list(psO[:len(Vts)]) + [psL])

        for i in range(n + LA):
            if i < n:
                rec_qk(i)
            if i >= LA:
                rec_pv(i - LA)
        cnt += n
        P.op("act", lambda e, tn=tn: e.activation(out=rl[:, 0:tn], in_=psL[:, 0:tn], func=AF.Copy),
             reads=[psL], writes=[rl])
        P.op("dve", lambda e, tn=tn: e.reciprocal(out=rl[:, 0:tn], in_=rl[:, 0:tn]), reads=[rl], writes=[rl])
        for vi in range(len(Vts)):
            P.op("act", lambda e, vi=vi, tn=tn: e.activation(out=of[:, 0:tn], in_=psO[vi][:, 0:tn], func=AF.Copy),
                 reads=[psO[vi]], writes=[of])
            P.op("dve", lambda e, vi=vi, t0=t0, tn=tn: e.tensor_tensor(
                out=outs[vi][:, t0:t0 + tn], in0=of[:, 0:tn], in1=rl[:, 0:tn], op=ALU.mult),
                reads=[of, rl], writes=[outs[vi]])
    return cnt


def build_even_b1(lambda_init):
    P = Prog()
    QT_d = P.dram("QT", [32, 128, T], BF16, "ExternalInput")
    KT_d = P.dram("KTall", [20, 128, NKEY], BF16, "ExternalInput")
    V_d = P.dram("Vall", [20, 128, NKB, 128], BF16, "ExternalInput")
    AT_d = P.dram("attnT", [32, 128, T], BF16, "ExternalOutput")
    C = load_consts(P, [("lamv", [128, 4]), ("subg", [128, 2])])
    scale = 1.0 / math.sqrt(128.0)
    onesb = P.sb("onesb", [128, 128], BF16)
    P.op("dve", lambda e: e.memset(onesb[:], 1.0), writes=[onesb])
    ones256 = P.sb("ones256", [128, 128], BF16)
    P.op("dve", lambda e: e.memset(ones256[:], 1.0 / 256.0), writes=[ones256])
    onesf = P.sb("onesf", [128, 128], F32)
    P.op("dve", lambda e: e.memset(onesf[:], 1.0), writes=[onesf])
    epsr = P.sb("epsr", [128, 1], F32)
    P.op("dve", lambda e: e.memset(epsr[:], RMS_EPS), writes=[epsr])
    ps_lam, ps_ss = P.psum[7], P.psum[7]
    prods = P.sb("prods", [128, 2], F32)
    ex = P.sb("ex", [128, 2], F32)
    neglam = P.sb("neglam", [128, 1], F32)
    lamv = C["lamv"]
    P.op("dve", lambda e: e.tensor_tensor(out=prods[:, 0:1], in0=lamv[:, 0:1], in1=lamv[:, 1:2], op=ALU.mult),
         reads=[lamv], writes=[prods])
    P.op("dve", lambda e: e.tensor_tensor(out=prods[:, 1:2], in0=lamv[:, 2:3], in1=lamv[:, 3:4], op=ALU.mult),
         reads=[lamv, prods], writes=[prods])
    P.op("pe", lambda e: e.matmul(ps_lam[:, 0:2], lhsT=onesf[:], rhs=prods[:], start=True, stop=True),
         reads=[onesf, prods], writes=[ps_lam])
    P.op("act", lambda e: e.activation(out=ex[:], in_=ps_lam[:, 0:2], func=AF.Exp), reads=[ps_lam], writes=[ex])
    P.op("dve", lambda e: e.tensor_tensor(out=neglam[:], in0=ex[:, 1:2], in1=ex[:, 0:1], op=ALU.subtract),
         reads=[ex], writes=[neglam])
    P.op("dve", lambda e: e.tensor_scalar(out=neglam[:], in0=neglam[:], scalar1=-float(lambda_init), scalar2=None,
                                          op0=ALU.add), reads=[neglam], writes=[neglam])
    gsc = P.sb("gsc", [128, 2], F32)
    P.op("dve", lambda e: e.tensor_scalar(out=gsc[:], in0=C["subg"][:], scalar1=float(1.0 - lambda_init),
                                          scalar2=None, op0=ALU.mult), reads=[C["subg"]], writes=[gsc])
    Kb = [P.sb("Kb%d" % i, [128, NKEY], BF16) for i in range(4)]
    Vb = [P.sb("Vb%d" % i, [128, NKB, 128], BF16) for i in range(4)]
    Qb = [P.sb("Qb%d" % i, [128, T], BF16) for i in range(4)]
    Pb = [P.sb("Pb%d" % i, [128, 512], BF16) for i in range(4)]
    A1 = [P.sb("A1%d" % i, [128, T], F32) for i in range(2)]
    A2 = [P.sb("A2%d" % i, [128, T], F32) for i in range(2)]
    sqb = [P.sb("sqb%d" % i, [128, 512], BF16) for i in range(2)]
    rl = P.sb("rl", [128, 512], F32)
    of = P.sb("of", [128, 512], F32)
    rs = P.sb("rs", [128, 512], F32)
    obs = [P.sb("obs%d" % i, [128, T], BF16) for i in range(4)]
    psS, psO, psL = P.psum[0:3] + P.psum[6:7], P.psum[3:5], P.psum[5]
    cnt = 0
    import os
    nheads = int(os.environ.get("K_NH", "8"))
    ngqa = int(os.environ.get("K_NG", "16"))
    for h in range(nheads):
        ia, ib = (2 * h) % 4, (2 * h + 1) % 4
        for ch, i4 in ((2 * h, ia), (2 * h + 1, ib)):
            P.dma("sp", Qb[i4][:], QT_d.ap()[ch], Qb[i4], writes=[Qb[i4]])
            P.dma("sp", Kb[i4][:], KT_d.ap()[ch], Kb[i4], writes=[Kb[i4]])
            P.dma("sp", Vb[i4][:], V_d.ap()[ch], Vb[i4], writes=[Vb[i4]])
        Vts = [Vb[ia], Vb[ib]]
        cnt = attn_unit(P, Qb[ia], Kb[ia], Vts, A1, Pb, psS, psO, psL, onesb, scale, rl, of, cnt)
        cnt = attn_unit(P, Qb[ib], Kb[ib], Vts, A2, Pb, psS, psO, psL, onesb, scale, rl, of, cnt)
        o0, o1 = obs[(2 * h) % 4], obs[(2 * h + 1) % 4]
        for vi in range(2):
            P.op("dve", lambda e, vi=vi: e.scalar_tensor_tensor(
                out=A1[vi][:], in0=A2[vi][:], scalar=neglam[:, 0:1], in1=A1[vi][:], op0=ALU.mult, op1=ALU.add),
                reads=[A2[vi], A1[vi], neglam], writes=[A1[vi]])
        for (t0, tn) in TT:
            for vi in range(2):
                P.op("act", lambda e, vi=vi, t0=t0, tn=tn: e.activation(
                    out=sqb[vi][:, 0:tn], in_=A1[vi][:, t0:t0 + tn], func=AF.Square), reads=[A1[vi]], writes=[sqb[vi]])

            def ssmm(e, tn=tn):
                e.matmul(ps_ss[:, 0:tn], lhsT=ones256[:], rhs=sqb[0][:, 0:tn], start=True, stop=False)
                return e.matmul(ps_ss[:, 0:tn], lhsT=ones256[:], rhs=sqb[1][:, 0:tn], start=False, stop=True)
            P.op("pe", ssmm, reads=[ones256, sqb[0], sqb[1]], writes=[ps_ss])
            P.op("act", lambda e, tn=tn: e.activation(out=rs[:, 0:tn], in_=ps_ss[:, 0:tn], func=AF.Sqrt,
                                                      bias=epsr[:, 0:1], scale=1.0), reads=[ps_ss, epsr], writes=[rs])
            P.op("dve", lambda e, tn=tn: e.reciprocal(out=rs[:, 0:tn], in_=rs[:, 0:tn]), reads=[rs], writes=[rs])
            for vi, ob in ((0, o0), (1, o1)):
                P.op("dve", lambda e, vi=vi, ob=ob, t0=t0, tn=tn: e.scalar_tensor_tensor(
                    out=ob[:, t0:t0 + tn], in0=A1[vi][:, t0:t0 + tn], scalar=gsc[:, vi:vi + 1], in1=rs[:, 0:tn],
                    op0=ALU.mult, op1=ALU.mult), reads=[A1[vi], gsc, rs], writes=[ob])
        P.dma("sp", AT_d.ap()[2 * h], o0[:], o0, reads=[o0])
        P.dma("sp", AT_d.ap()[2 * h + 1], o1[:], o1, reads=[o1])
    for g in range(ngqa):
        kv = g // 4
        if g % 4 == 0:
            P.dma("sp", Kb[kv % 4][:], KT_d.ap()[16 + kv], Kb[kv % 4], writes=[Kb[kv % 4]])
            P.dma("sp", Vb[kv % 4][:], V_d.ap()[16 + kv], Vb[kv % 4], writes=[Vb[kv % 4]])
        P.dma("sp", Qb[g % 4][:], QT_d.ap()[16 + g], Qb[g % 4], writes=[Qb[g % 4]])
        cnt = attn_unit(P, Qb[g % 4], Kb[kv % 4], [Vb[kv % 4]], [A1[g % 2]], Pb, psS, psO, psL, onesb, scale,
                        rl, of, cnt)
        ob = obs[g % 4]
        P.op("act", lambda e, g=g, ob=ob: e.activation(out=ob[:], in_=A1[g % 2][:], func=AF.Copy),
             reads=[A1[g % 2]], writes=[ob])
        P.dma("sp", AT_d.ap()[16 + g], ob[:], ob, reads=[ob])
    return P.finish()


NEXP = 16
DEXP = 384
TOK_TILES = [(i * 128, 128) for i in range(LAT // 128)] + [(LAT, CT)]


def ln_stats_accum(P, yb, t0, tn, ti, oc, noc, S1, S2, ybf, ysq, onesb):
    P.op("act", lambda e: e.activation(out=ybf[:, 0:tn], in_=yb[:, t0:t0 + tn], func=AF.Copy),
         reads=[yb], writes=[ybf])
    P.op("act", lambda e: e.activation(out=ysq[:, 0:tn], in_=yb[:, t0:t0 + tn], func=AF.Square),
         reads=[yb], writes=[ysq])
    s1t, s1c = S1[ti]
    s2t, s2c = S2[ti]

    def mm(e):
        e.matmul(s1t[:, s1c:s1c + tn], lhsT=onesb[:], rhs=ybf[:, 0:tn], start=(oc == 0), stop=(oc == noc - 1))
        return e.matmul(s2t[:, s2c:s2c + tn], lhsT=onesb[:], rhs=ysq[:, 0:tn], start=(oc == 0), stop=(oc == noc - 1))
    P.op("pe", mm, reads=[onesb, ybf, ysq], writes=[s1t, s2t])


def ln_finalize(P, S1, S2, mu, rstd, nmr, tmp, epsl):
    for ti, (t0, tn) in enumerate(TT):
        s1t, s1c = S1[ti]
        s2t, s2c = S2[ti]
        P.op("act", lambda e, s1t=s1t, s1c=s1c, t0=t0, tn=tn: e.activation(
            out=mu[:, t0:t0 + tn], in_=s1t[:, s1c:s1c + tn], func=AF.Copy, scale=1.0 / D), reads=[s1t], writes=[mu])
        P.op("act", lambda e, s2t=s2t, s2c=s2c, t0=t0, tn=tn: e.activation(
            out=rstd[:, t0:t0 + tn], in_=s2t[:, s2c:s2c + tn], func=AF.Copy, scale=1.0 / D), reads=[s2t], writes=[rstd])
    P.op("dve", lambda e: e.tensor_tensor(out=tmp[:], in0=mu[:], in1=mu[:], op=ALU.mult), reads=[mu], writes=[tmp])
    P.op("dve", lambda e: e.tensor_tensor(out=rstd[:], in0=rstd[:], in1=tmp[:], op=ALU.subtract),
         reads=[rstd, tmp], writes=[rstd])
    P.op("act", lambda e: e.activation(out=rstd[:], in_=rstd[:], func=AF.Sqrt, bias=epsl[:, 0:1], scale=1.0),
         reads=[rstd, epsl], writes=[rstd])
    P.op("dve", lambda e: e.reciprocal(out=rstd[:], in_=rstd[:]), reads=[rstd], writes=[rstd])
    P.op("dve", lambda e: e.tensor_tensor(out=nmr[:], in0=mu[:], in1=rstd[:], op=ALU.mult), reads=[mu, rstd], writes=[nmr])


def build_post():
    P = Prog()
    AT_d = P.dram("attnT", [32, 128, T], BF16, "ExternalInput")
    xT_d = P.dram("xT", [D, T], F32, "ExternalInput")
    wo_d = P.dram("w_out", [D, D], F32, "ExternalInput")
    wg_d = P.dram("w_gate", [NEXP, D, DEXP], F32, "ExternalInput")
    wu_d = P.dram("w_up", [NEXP, D, DEXP], F32, "ExternalInput")
    wd_d = P.dram("w_down", [NEXP * DEXP, D], F32, "ExternalInput")
    wgr_d = P.dram("wgr", [D, 20], F32, "ExternalInput")
    xo_d = P.dram("xT_new", [D, T], F32, "ExternalOutput")
    y1_d = P.dram("y1T", [D, T], F32, "Internal")
    x1_d = P.dram("x1T", [D, T], F32, "Internal")
    fp_d = P.dram("fpart", [D, T], F32, "Internal")
    y2_d = P.dram("y2T", [D, T], F32, "Internal")
    C = load_consts(P, [("g1", [128, KC, 2]), ("sc2", [128, KC, 2]), ("sh2", [128, KC, 2]), ("g2", [128, KC, 2]),
                        ("ln1g", [128, KC]), ("ln1b", [128, KC]), ("ln2g", [128, KC]), ("ln2b", [128, KC]),
                        ("bgr", [128, 20]), ("identf", [128, 128]), ("selm", [16, NEXP * 128])])
    onesb = P.sb("onesb", [128, 128], BF16)
    P.op("dve", lambda e: e.memset(onesb[:], 1.0), writes=[onesb])
    epsl = P.sb("epsl", [128, 1], F32)
    P.op("dve", lambda e: e.memset(epsl[:], LN_EPS), writes=[epsl])
    sc2p = P.sb("sc2p", [128, KC, 2], F32)
    P.op("dve", lambda e: e.tensor_scalar(out=sc2p[:], in0=C["sc2"][:], scalar1=1.0, scalar2=None, op0=ALU.add),
         reads=[C["sc2"]], writes=[sc2p])
    mu = P.sb("mu", [128, T], F32)
    rstd = P.sb("rstd", [128, T], F32)
    nmr = P.sb("nmr", [128, T], F32)
    tmpT = P.sb("tmpT", [128, T], F32)
    wbufs = [P.sb("wb%d" % i, [128, KC, 128], BF16) for i in range(3)]
    xbufs = [P.sb("xb%d" % i, [128, T], F32) for i in range(2)]
    ybufs = [P.sb("yb%d" % i, [128, T], F32) for i in range(2)]
    of = [P.sb("of0", [128, 512], F32)] * 2
    ybf = P.sb("ybf", [128, 512], BF16)
    ysq = P.sb("ysq", [128, 512], BF16)
    ps3 = P.psum[0:3]
    S1 = [(P.psum[3], 0), (P.psum[4], 0), (P.psum[7], 0)]
    S2 = [(P.psum[5], 0), (P.psum[6], 0), (P.psum[7], 64)]
    h2T = P.sb("h2T", [128, KC, T], BF16)
    m_phase = P.mark()

    def residual_epilogue(oc, gmod, src_x_d, dst_y_d, extra_d=None):
        xb = xbufs[oc % 2]
        yb = ybufs[oc % 2]
        P.dma("sp", xb[:], src_x_d.ap()[oc * 128:(oc + 1) * 128, :], xb, writes=[xb])
        P.op("act", lambda e: e.activation(out=xb[:], in_=xb[:], func=AF.Copy, scale=float(ALPHA)),
             reads=[xb], writes=[xb])
        if extra_d is not None:
            P.dma("sp", yb[:], extra_d.ap()[oc * 128:(oc + 1) * 128, :], yb, writes=[yb])
        for ti, (t0, tn) in enumerate(TT):
            o = of[ti % 2]
            P.op("act", lambda e, ti=ti, tn=tn, o=o: e.activation(out=o[:, 0:tn], in_=ps3[ti][:, 0:tn], func=AF.Copy),
                 reads=[ps3[ti]], writes=[o])
            if extra_d is not None:
                P.op("dve", lambda e, t0=t0, tn=tn, o=o: e.tensor_tensor(
                    out=o[:, 0:tn], in0=o[:, 0:tn], in1=yb[:, t0:t0 + tn], op=ALU.add), reads=[o, yb], writes=[o])
            col = 0 if ti < 2 else 1
            P.op("dve", lambda e, t0=t0, tn=tn, o=o, col=col: e.scalar_tensor_tensor(
                out=yb[:, t0:t0 + tn], in0=o[:, 0:tn], scalar=gmod[:, oc, col:col + 1], in1=xb[:, t0:t0 + tn],
                op0=ALU.mult, op1=ALU.add), reads=[o, gmod, xb], writes=[yb])
            ln_stats_accum(P, yb, t0, tn, ti, oc, KC, S1, S2, ybf, ysq, onesb)
        P.dma("sp", dst_y_d.ap()[oc * 128:(oc + 1) * 128, :], yb[:], yb, reads=[yb])

    def ln_apply(src_d, g, b, dst_d, with_h2):
        for oc in range(KC):
            yb = ybufs[oc % 2]
            xb = xbufs[oc % 2]
            P.dma("sp", yb[:], src_d.ap()[oc * 128:(oc + 1) * 128, :], yb, writes=[yb])
            P.op("dve", lambda e, yb=yb: e.tensor_tensor(out=yb[:], in0=yb[:], in1=rstd[:], op=ALU.mult),
                 reads=[yb, rstd], writes=[yb])
            P.op("dve", lambda e, yb=yb: e.tensor_tensor(out=yb[:], in0=yb[:], in1=nmr[:], op=ALU.subtract),
                 reads=[yb, nmr], writes=[yb])
            P.op("act", lambda e, yb=yb, xb=xb, oc=oc: e.activation(
                out=xb[:], in_=yb[:], func=AF.Identity, bias=b[:, oc:oc + 1], scale=g[:, oc:oc + 1]),
                reads=[yb, g, b], writes=[xb])
            P.dma("sp", dst_d.ap()[oc * 128:(oc + 1) * 128, :], xb[:], xb, reads=[xb])
            if with_h2:
                def f(e, xb=xb, oc=oc):
                    e.activation(out=h2T[:, oc, 0:LAT], in_=xb[:, 0:LAT], func=AF.Identity,
                                 bias=C["sh2"][:, oc, 0:1], scale=sc2p[:, oc, 0:1])
                    return e.activation(out=h2T[:, oc, LAT:T], in_=xb[:, LAT:T], func=AF.Identity,
                                        bias=C["sh2"][:, oc, 1:2], scale=sc2p[:, oc, 1:2])
                P.op("act", f, reads=[xb, sc2p, C["sh2"]], writes=[h2T])

    aT = P.sb("aT", [128, KC, T], BF16)
    for ch in range(32):
        P.dma("sp", aT[:, ch, :], AT_d.ap()[ch], aT, writes=[aT])
    for oc in range(KC):
        gemm_fm(P, aT, KC, wo_d, oc * 128, 128, wbufs, ps3, oc)
        residual_epilogue(oc, C["g1"], xT_d, y1_d)
    ln_finalize(P, S1, S2, mu, rstd, nmr, tmpT, epsl)
    P.fence()
    P.release(m_phase)
    ln_apply(y1_d, C["ln1g"], C["ln1b"], x1_d, True)
    P.fence()
    wgrb = P.sb("wgrb", [128, KC, 20], BF16)
    P.dma("pool", wgrb[:], wgr_d.ap().rearrange("(kc p) n -> p kc n", p=128), wgrb, writes=[wgrb])
    gT = P.sb("gT", [16, T], F32)
    lg = P.sb("lg", [128, 20], F32)
    sm = {n: P.sb("r_" + n, [128, w], F32) for n, w in (
        ("gmax", 1), ("ngmax", 1), ("ohg", 4), ("ge", 4), ("gsum", 1), ("gw", 1), ("el", 4), ("m1", 1), ("oh1", 4),
        ("el2", 4), ("m2", 1), ("oh2", 4), ("d", 1), ("e2", 1), ("den", 1), ("p1", 1), ("p2", 1), ("w1", 1),
        ("w2", 1), ("gin", 4), ("gates", 16))}
    ps_l, ps_t = P.psum[0], P.psum[1]
    AX = mybir.AxisListType.X
    for (t0, tn) in TOK_TILES:
        pr = slice(0, tn)

        def lmm(e, t0=t0, tn=tn):
            ins = None
            for kc in range(KC):
                ins = e.matmul(ps_l[0:tn, 0:20], lhsT=h2T[:, kc, t0:t0 + tn], rhs=wgrb[:, kc, :],
                               start=(kc == 0), stop=(kc == KC - 1))
            return ins
        P.op("pe", lmm, reads=[h2T, wgrb], writes=[ps_l])
        P.op("act", lambda e, pr=pr: e.activation(out=lg[pr, :], in_=ps_l[pr, 0:20], func=AF.Copy),
             reads=[ps_l], writes=[lg])
        P.op("dve", lambda e, pr=pr: e.tensor_tensor(out=lg[pr, :], in0=lg[pr, :], in1=C["bgr"][pr, :], op=ALU.add),
             reads=[lg, C["bgr"]], writes=[lg])

        def dv(fn, reads, writes):
            P.op("dve", fn, reads=[sm[r] if isinstance(r, str) else r for r in reads],
                 writes=[sm[w] for w in writes])
        dv(lambda e, pr=pr: e.reduce_max(out=sm["gmax"][pr, :], in_=lg[pr, 0:4], axis=AX), [lg], ["gmax"])
        dv(lambda e, pr=pr: e.tensor_scalar(out=sm["ohg"][pr, :], in0=lg[pr, 0:4], scalar1=sm["gmax"][pr, 0:1],
                                            scalar2=None, op0=ALU.is_ge), [lg, "gmax"], ["ohg"])
        dv(lambda e, pr=pr: e.tensor_scalar(out=sm["ngmax"][pr, :], in0=sm["gmax"][pr, :], scalar1=-1.0,
                                            scalar2=None, op0=ALU.mult), ["gmax"], ["ngmax"])
        P.op("act", lambda e, pr=pr: e.activation(out=sm["ge"][pr, :], in_=lg[pr, 0:4], func=AF.Exp,
                                                  bias=sm["ngmax"][pr, 0:1], scale=1.0),
             reads=[lg, sm["ngmax"]], writes=[sm["ge"]])
        dv(lambda e, pr=pr: e.reduce_sum(out=sm["gsum"][pr, :], in_=sm["ge"][pr, :], axis=AX), ["ge"], ["gsum"])
        dv(lambda e, pr=pr: e.reciprocal(out=sm["gw"][pr, :], in_=sm["gsum"][pr, :]), ["gsum"], ["gw"])
        dv(lambda e, pr=pr: e.tensor_scalar(out=sm["el"][pr, :], in0=lg[pr, 4:8], scalar1=sm["ohg"][pr, 0:1],
                                            scalar2=None, op0=ALU.mult), [lg, "ohg"], ["el"])
        for g in range(1, 4):
            dv(lambda e, pr=pr, g=g: e.scalar_tensor_tensor(
                out=sm["el"][pr, :], in0=lg[pr, 4 + 4 * g:8 + 4 * g], scalar=sm["ohg"][pr, g:g + 1],
                in1=sm["el"][pr, :], op0=ALU.mult, op1=ALU.add), [lg, "ohg", "el"], ["el"])
        dv(lambda e, pr=pr: e.reduce_max(out=sm["m1"][pr, :], in_=sm["el"][pr, :], axis=AX), ["el"], ["m1"])
        dv(lambda e, pr=pr: e.tensor_scalar(out=sm["oh1"][pr, :], in0=sm["el"][pr, :], scalar1=sm["m1"][pr, 0:1],
                                            scalar2=None, op0=ALU.is_ge), ["el", "m1"], ["oh1"])
        dv(lambda e, pr=pr: e.scalar_tensor_tensor(out=sm["el2"][pr, :], in0=sm["oh1"][pr, :], scalar=-1e30,
                                                   in1=sm["el"][pr, :], op0=ALU.mult, op1=ALU.add),
           ["oh1", "el"], ["el2"])
        dv(lambda e, pr=pr: e.reduce_max(out=sm["m2"][pr, :], in_=sm["el2"][pr, :], axis=AX), ["el2"], ["m2"])
        dv(lambda e, pr=pr: e.tensor_scalar(out=sm["oh2"][pr, :], in0=sm["el2"][pr, :], scalar1=sm["m2"][pr, 0:1],
                                            scalar2=None, op0=ALU.is_ge), ["el2", "m2"], ["oh2"])
        dv(lambda e, pr=pr: e.tensor_tensor(out=sm["d"][pr, :], in0=sm["m2"][pr, :], in1=sm["m1"][pr, :],
                                            op=ALU.subtract), ["m2", "m1"], ["d"])
        P.op("act", lambda e, pr=pr: e.activation(out=sm["e2"][pr, :], in_=sm["d"][pr, :], func=AF.Exp),
             reads=[sm["d"]], writes=[sm["e2"]])
        dv(lambda e, pr=pr: e.tensor_scalar(out=sm["den"][pr, :], in0=sm["e2"][pr, :], scalar1=1.0, scalar2=None,
                                            op0=ALU.add), ["e2"], ["den"])
        dv(lambda e, pr=pr: e.reciprocal(out=sm["p1"][pr, :], in_=sm["den"][pr, :]), ["den"], ["p1"])
        dv(lambda e, pr=pr: e.tensor_tensor(out=sm["p2"][pr, :], in0=sm["e2"][pr, :], in1=sm["p1"][pr, :],
                                            op=ALU.mult), ["e2", "p1"], ["p2"])
        dv(lambda e, pr=pr: e.tensor_tensor(out=sm["w1"][pr, :], in0=sm["p1"][pr, :], in1=sm["gw"][pr, :],
                                            op=ALU.mult), ["p1", "gw"], ["w1"])
        dv(lambda e, pr=pr: e.tensor_tensor(out=sm["w2"][pr, :], in0=sm["p2"][pr, :], in1=sm["gw"][pr, :],
                                            op=ALU.mult), ["p2", "gw"], ["w2"])
        dv(lambda e, pr=pr: e.tensor_scalar(out=sm["gin"][pr, :], in0=sm["oh1"][pr, :], scalar1=sm["w1"][pr, 0:1],
                                            scalar2=None, op0=ALU.mult), ["oh1", "w1"], ["gin"])
        dv(lambda e, pr=pr: e.scalar_tensor_tensor(out=sm["gin"][pr, :], in0=sm["oh2"][pr, :],
                                                   scalar=sm["w2"][pr, 0:1], in1=sm["gin"][pr, :],
                                                   op0=ALU.mult, op1=ALU.add), ["oh2", "w2", "gin"], ["gin"])
        for g in range(4):
            dv(lambda e, pr=pr, g=g: e.tensor_scalar(out=sm["gates"][pr, 4 * g:4 * g + 4], in0=sm["gin"][pr, :],
                                                     scalar1=sm["ohg"][pr, g:g + 1], scalar2=None, op0=ALU.mult),
               ["gin", "ohg", "gates"], ["gates"])
        P.op("pe", lambda e, pr=pr, tn=tn: e.matmul(ps_t[0:16, 0:tn], lhsT=sm["gates"][pr, :],
                                                    rhs=C["identf"][pr, 0:tn], start=True, stop=True),
             reads=[sm["gates"], C["identf"]], writes=[ps_t])
        P.op("act", lambda e, t0=t0, tn=tn: e.activation(out=gT[:, t0:t0 + tn], in_=ps_t[0:16, 0:tn], func=AF.Copy),
             reads=[ps_t], writes=[gT])
    hidT = P.sb("hidT", [128, 24, T], BF16)
    gbc = P.sb("gbc", [128, T], F32)
    sg = [P.sb("sg%d" % i, [128, 512], F32) for i in range(2)]
    uu = [P.sb("uu0", [128, 512], F32)] * 2
    psG, psU, ps_b = P.psum[0:3], P.psum[3:6], P.psum[6]
    widx = 0
    for half in range(2):
        for el in range(8):
            ex = half * 8 + el
            for ti, (t0, tn) in enumerate(TT):
                P.op("pe", lambda e, ex=ex, t0=t0, tn=tn: e.matmul(
                    ps_b[:, 0:tn], lhsT=C["selm"][:, ex * 128:(ex + 1) * 128], rhs=gT[:, t0:t0 + tn],
                    start=True, stop=True), reads=[C["selm"], gT], writes=[ps_b])
                P.op("act", lambda e, t0=t0, tn=tn: e.activation(out=gbc[:, t0:t0 + tn], in_=ps_b[:, 0:tn], func=AF.Copy),
                     reads=[ps_b], writes=[gbc])
            for fc in range(3):
                gemm_fm(P, h2T, KC, wg_d.ap()[ex], fc * 128, 128, wbufs, psG, widx)
                widx += 1
                gemm_fm(P, h2T, KC, wu_d.ap()[ex], fc * 128, 128, wbufs, psU, widx)
                widx += 1
                for ti, (t0, tn) in enumerate(TT):
                    j = ti % 2
                    P.op("act", lambda e, ti=ti, tn=tn, j=j: e.activation(out=sg[j][:, 0:tn], in_=psG[ti][:, 0:tn], func=AF.Silu),
                         reads=[psG[ti]], writes=[sg[j]])
                    P.op("act", lambda e, ti=ti, tn=tn, j=j: e.activation(out=uu[j][:, 0:tn], in_=psU[ti][:, 0:tn], func=AF.Copy),
                         reads=[psU[ti]], writes=[uu[j]])
                    P.op("dve", lambda e, tn=tn, j=j: e.tensor_tensor(out=sg[j][:, 0:tn], in0=sg[j][:, 0:tn], in1=uu[j][:, 0:tn], op=ALU.mult),
                         reads=[sg[j], uu[j]], writes=[sg[j]])
                    P.op("dve", lambda e, t0=t0, tn=tn, j=j, el=el, fc=fc: e.tensor_tensor(
                        out=hidT[:, el * 3 + fc, t0:t0 + tn], in0=sg[j][:, 0:tn], in1=gbc[:, t0:t0 + tn], op=ALU.mult),
                        reads=[sg[j], gbc], writes=[hidT])
        wd_half = wd_d.ap()[half * 8 * DEXP:(half + 1) * 8 * DEXP, :]
        for oc in range(KC):
            gemm_fm(P, hidT, 24, wd_half, oc * 128, 128, wbufs, ps3, widx)
            widx += 1
            if half == 0:
                yb = ybufs[oc % 2]
                for ti, (t0, tn) in enumerate(TT):
                    P.op("act", lambda e, ti=ti, t0=t0, tn=tn, yb=yb: e.activation(
                        out=yb[:, t0:t0 + tn], in_=ps3[ti][:, 0:tn], func=AF.Copy), reads=[ps3[ti]], writes=[yb])
                P.dma("sp", fp_d.ap()[oc * 128:(oc + 1) * 128, :], yb[:], yb, reads=[yb])
            else:
                residual_epilogue(oc, C["g2"], x1_d, y2_d, extra_d=fp_d)
        P.fence()
    ln_finalize(P, S1, S2, mu, rstd, nmr, tmpT, epsl)
    P.fence()
    ln_apply(y2_d, C["ln2g"], C["ln2b"], xo_d, False)
    return P.finish()


def sel_matrix():
    m = np.zeros((16, NEXP, 128), np.float32)
    for e in range(NEXP):
        m[e, e, :] = 1.0
    return np.ascontiguousarray(m.reshape(16, NEXP * 128))


OD_IN = 1600
NH = 32


def rope_tile(P, ps, nrow, t0, tn, j, qn, qb, t1, t2, rot, cosT, sinT, ps_r, ob):
    pr = slice(0, nrow)
    P.op("act", lambda e: e.activation(out=qn[j][pr, 0:tn], in_=ps[pr, 0:tn], func=AF.Copy), reads=[ps], writes=[qn[j]])
    P.op("act", lambda e: e.activation(out=qb[j][pr, 0:tn], in_=qn[j][pr, 0:tn], func=AF.Copy), reads=[qn[j]], writes=[qb[j]])
    P.op("pe", lambda e: e.matmul(ps_r[pr, 0:tn], lhsT=rot[pr, 0:nrow], rhs=qb[j][pr, 0:tn], start=True, stop=True),
         reads=[rot, qb[j]], writes=[ps_r])
    P.op("act", lambda e: e.activation(out=t2[j][pr, 0:tn], in_=ps_r[pr, 0:tn], func=AF.Copy), reads=[ps_r], writes=[t2[j]])
    P.op("dve", lambda e: e.tensor_tensor(out=t2[j][pr, 0:tn], in0=t2[j][pr, 0:tn], in1=sinT[pr, t0:t0 + tn], op=ALU.mult),
         reads=[t2[j], sinT], writes=[t2[j]])
    P.op("dve", lambda e: e.tensor_tensor(out=t1[j][pr, 0:tn], in0=qn[j][pr, 0:tn], in1=cosT[pr, t0:t0 + tn], op=ALU.mult),
         reads=[qn[j], cosT], writes=[t1[j]])
    P.op("dve", lambda e: e.tensor_tensor(out=ob[pr, t0:t0 + tn], in0=t1[j][pr, 0:tn], in1=t2[j][pr, 0:tn], op=ALU.add),
         reads=[t1[j], t2[j]], writes=[ob])


def build_odd_a():
    P = Prog()
    xT_d = P.dram("xT", [D, T], F32, "ExternalInput")
    w_d = P.dram("w_in", [D, OD_IN], F32, "ExternalInput")
    wuq_d = P.dram("w_uq", [1024, NH * 192], F32, "ExternalInput")
    wukv_d = P.dram("w_ukv", [512, NH * 256], F32, "ExternalInput")
    QN_d = P.dram("QN", [NH, 128, T], BF16, "ExternalOutput")
    QR_d = P.dram("QR", [NH, 64, T], BF16, "ExternalOutput")
    KN_d = P.dram("KN", [NH, 128, T], BF16, "ExternalOutput")
    KR_d = P.dram("KR", [64, T], BF16, "ExternalOutput")
    VT_d = P.dram("VT", [NH, 128, T], BF16, "ExternalOutput")
    C = load_consts(P, [("sc", [128, KC, 2]), ("sh", [128, KC, 2]), ("cosT", [64, T]), ("sinT", [64, T]),
                        ("gqn", [128, 8]), ("gkvn", [128, 4])])
    rot = make_rot(P, "rotm", 64)
    onesq = P.sb("onesq", [128, 128], BF16)
    P.op("dve", lambda e: e.memset(onesq[:], 1.0 / 1024.0), writes=[onesq])
    onesk = P.sb("onesk", [128, 128], BF16)
    P.op("dve", lambda e: e.memset(onesk[:], 1.0 / 512.0), writes=[onesk])
    epsr = P.sb("epsr", [128, 1], F32)
    P.op("dve", lambda e: e.memset(epsr[:], RMS_EPS), writes=[epsr])
    sc1p = P.sb("sc1p", [128, KC, 2], F32)
    P.op("dve", lambda e: e.tensor_scalar(out=sc1p[:], in0=C["sc"][:], scalar1=1.0, scalar2=None, op0=ALU.add),
         reads=[C["sc"]], writes=[sc1p])
    cq32 = P.sb("cq32", [128, 8, T], F32)
    ckv32 = P.sb("ckv32", [128, 4, T], F32)
    rsq = P.sb("rsq", [128, T], F32)
    rskv = P.sb("rskv", [128, T], F32)
    wbufs = [P.sb("wb%d" % i, [128, KC, 128], BF16) for i in range(3)]
    obufs = [P.sb("ob%d" % i, [128, T], BF16) for i in range(3)]
    qb = [P.sb("qb%d" % i, [128, 512], BF16) for i in range(2)]
    sq = P.sb("sq", [128, 512], BF16)
    qn = [P.sb("qn%d" % i, [128, 512], F32) for i in range(2)]
    t1 = [P.sb("t1%d" % i, [128, 512], F32) for i in range(2)]
    t2 = [P.sb("t2%d" % i, [128, 512], F32) for i in range(2)]
    ps3 = P.psum[0:3]
    Sq = [(P.psum[3], 0), (P.psum[4], 0), (P.psum[7], 0)]
    Sk = [(P.psum[5], 0), (P.psum[6], 0), (P.psum[7], 64)]
    m_phase = P.mark()
    hT = P.sb("hT", [128, KC, T], BF16)
    xbufs = [P.sb("xb%d" % i, [128, T], F32) for i in range(2)]
    modulate_load(P, xT_d, sc1p, C["sh"], hT, xbufs)
    widx = 0
    for jj in range(12):
        isq = jj < 8
        dst, jdx, S, ones_, nch = (cq32, jj, Sq, onesq, 8) if isq else (ckv32, jj - 8, Sk, onesk, 4)
        gemm_fm(P, hT, KC, w_d, jj * 128, 128, wbufs, ps3, widx)
        widx += 1
        for ti, (t0, tn) in enumerate(TT):
            P.op("act", lambda e, ti=ti, t0=t0, tn=tn, dst=dst, jdx=jdx: e.activation(
                out=dst[:, jdx, t0:t0 + tn], in_=ps3[ti][:, 0:tn], func=AF.Copy), reads=[ps3[ti]], writes=[dst])
            P.op("act", lambda e, ti=ti, tn=tn: e.activation(out=sq[:, 0:tn], in_=ps3[ti][:, 0:tn], func=AF.Square),
                 reads=[ps3[ti]], writes=[sq])
            st, scol = S[ti]
            P.op("pe", lambda e, st=st, scol=scol, tn=tn, ones_=ones_, jdx=jdx, nch=nch: e.matmul(
                st[:, scol:scol + tn], lhsT=ones_[:], rhs=sq[:, 0:tn], start=(jdx == 0), stop=(jdx == nch - 1)),
                reads=[ones_, sq], writes=[st])
    for S, rs_ in ((Sq, rsq), (Sk, rskv)):
        for ti, (t0, tn) in enumerate(TT):
            st, scol = S[ti]
            P.op("act", lambda e, st=st, scol=scol, t0=t0, tn=tn, rs_=rs_: e.activation(
                out=rs_[:, t0:t0 + tn], in_=st[:, scol:scol + tn], func=AF.Sqrt, bias=epsr[:, 0:1], scale=1.0),
                reads=[st, epsr], writes=[rs_])
        P.op("dve", lambda e, rs_=rs_: e.reciprocal(out=rs_[:], in_=rs_[:]), reads=[rs_], writes=[rs_])
    ps_r = P.psum[3]
    gemm_fm(P, hT, KC, w_d, 1536, 64, wbufs, ps3, widx)
    widx += 1
    ob = obufs[0]
    for ti, (t0, tn) in enumerate(TT):
        rope_tile(P, ps3[ti], 64, t0, tn, ti % 2, qn, qb, t1, t2, rot, C["cosT"], C["sinT"], ps_r, ob)
    P.dma("sp", KR_d.ap(), ob[0:64, :], ob, reads=[ob])
    P.fence()
    P.release(m_phase)
    cqn = P.sb("cqn", [128, 8, T], BF16)
    ckvn = P.sb("ckvn", [128, 4, T], BF16)
    for j in range(8):
        P.op("dve", lambda e, j=j: e.scalar_tensor_tensor(out=cqn[:, j, :], in0=cq32[:, j, :], scalar=C["gqn"][:, j:j + 1],
                                                          in1=rsq[:], op0=ALU.mult, op1=ALU.mult),
             reads=[cq32, C["gqn"], rsq], writes=[cqn])
    for j in range(4):
        P.op("dve", lambda e, j=j: e.scalar_tensor_tensor(out=ckvn[:, j, :], in0=ckv32[:, j, :], scalar=C["gkvn"][:, j:j + 1],
                                                          in1=rskv[:], op0=ALU.mult, op1=ALU.mult),
             reads=[ckv32, C["gkvn"], rskv], writes=[ckvn])
    import os
    nh = int(os.environ.get("K_NH", str(NH)))
    oi = 0
    for h in range(nh):
        for (src, nk, wdr, col0, ncol, kind, od) in (
                (cqn, 8, wuq_d, h * 192, 128, "plain", QN_d), (cqn, 8, wuq_d, h * 192 + 128, 64, "rope", QR_d),
                (ckvn, 4, wukv_d, h * 256, 128, "plain", KN_d), (ckvn, 4, wukv_d, h * 256 + 128, 128, "plain", VT_d)):
            gemm_fm(P, src, nk, wdr, col0, ncol, wbufs, ps3, widx)
            widx += 1
            ob = obufs[oi % 3]
            oi += 1
            for ti, (t0, tn) in enumerate(TT):
                if kind == "plain":
                    P.op("act", lambda e, ti=ti, t0=t0, tn=tn, ob=ob: e.activation(
                        out=ob[:, t0:t0 + tn], in_=ps3[ti][:, 0:tn], func=AF.Copy), reads=[ps3[ti]], writes=[ob])
                else:
                    rope_tile(P, ps3[ti], 64, t0, tn, ti % 2, qn, qb, t1, t2, rot, C["cosT"], C["sinT"], ps_r, ob)
            P.dma("sp", od.ap()[h], ob[0:ncol, :], ob, reads=[ob])
    return P.finish()


def build_odd_b1():
    P = Prog()
    QN_d = P.dram("QN", [NH, 128, T], BF16, "ExternalInput")
    QR_d = P.dram("QR", [NH, 64, T], BF16, "ExternalInput")
    KN_d = P.dram("KNall", [NH, 128, NKEY], BF16, "ExternalInput")
    KR_d = P.dram("KRall", [64, NKEY], BF16, "ExternalInput")
    V_d = P.dram("Vall", [NH, 128, NKB, 128], BF16, "ExternalInput")
    AT_d = P.dram("attnT", [NH, 128, T], BF16, "ExternalOutput")
    scale = 1.0 / math.sqrt(192.0)
    onesb = P.sb("onesb", [128, 128], BF16)
    P.op("dve", lambda e: e.memset(onesb[:], 1.0), writes=[onesb])
    KR = P.sb("KR", [64, NKEY], BF16)
    P.dma("sp", KR[:], KR_d.ap(), KR, writes=[KR])
    Kb = [P.sb("Kb%d" % i, [128, NKEY], BF16) for i in range(3)]
    Vb = [P.sb("Vb%d" % i, [128, NKB, 128], BF16) for i in range(3)]
    Qb = [P.sb("Qb%d" % i, [128, T], BF16) for i in range(3)]
    Qr = [P.sb("Qr%d" % i, [64, T], BF16) for i in range(3)]
    Pb = [P.sb("Pb%d" % i, [128, 512], BF16) for i in range(6)]
    A1 = [P.sb("A1%d" % i, [128, T], F32) for i in range(2)]
    rl = P.sb("rl", [128, 512], F32)
    of = P.sb("of", [128, 512], F32)
    obs = [P.sb("obs%d" % i, [128, T], BF16) for i in range(3)]
    psS, psO, psL = P.psum[0:3] + P.psum[4:5] + P.psum[6:8], P.psum[3:4], P.psum[5]
    import os
    nh = int(os.environ.get("K_NH", str(NH)))
    cnt = 0
    for h in range(nh):
        i3 = h % 3
        P.dma("sp", Qb[i3][:], QN_d.ap()[h], Qb[i3], writes=[Qb[i3]])
        P.dma("sp", Qr[i3][:], QR_d.ap()[h], Qr[i3], writes=[Qr[i3]])
        P.dma("sp", Kb[i3][:], KN_d.ap()[h], Kb[i3], writes=[Kb[i3]])
        P.dma("sp", Vb[i3][:], V_d.ap()[h], Vb[i3], writes=[Vb[i3]])
        cnt = attn_unit(P, Qb[i3], Kb[i3], [Vb[i3]], [A1[h % 2]], Pb, psS, psO, psL, onesb, scale, rl, of, cnt,
                        Qt2=Qr[i3], Kt2=KR)
        ob = obs[i3]
        P.op("act", lambda e, h=h, ob=ob: e.activation(out=ob[:], in_=A1[h % 2][:], func=AF.Copy),
             reads=[A1[h % 2]], writes=[ob])
        P.dma("sp", AT_d.ap()[h], ob[:], ob, reads=[ob])
    return P.finish()


_CACHE = {}


def _get(name, builder, *args):
    key = (name,) + tuple(args)
    if key not in _CACHE:
        _CACHE[key] = builder(*args)
    return _CACHE[key]


def _f32(a):
    return np.ascontiguousarray(np.asarray(a, dtype=np.float32))


def _mods_all(c, c_ctx, w_ada, b_ada):
    nc = _get("mods", build_mods)
    cT = np.ascontiguousarray(np.stack([c[0], c_ctx], -1).reshape(KC, 128, 2).transpose(1, 0, 2))
    in_maps = []
    for j in range(NCORES):
        wad = np.ascontiguousarray(w_ada[:, :, j * MCOLS:(j + 1) * MCOLS])
        bad = np.ascontiguousarray(np.broadcast_to(
            b_ada[:, j * MCOLS:(j + 1) * MCOLS].reshape(1, -1), (2, DEPTH * MCOLS)))
        in_maps.append({"cT": cT, "wad": wad, "bad": bad})
    res = run(nc, in_maps)
    mo = np.concatenate([r["mo"].reshape(2, DEPTH, MCOLS) for r in res], axis=2)
    return mo[0].reshape(DEPTH, 6, D), mo[1].reshape(DEPTH, 6, D)


def _gather_keys(parts):
    lat = np.concatenate([p[:, :, 0:LAT] for p in parts], axis=2)
    ctx = np.concatenate([p[:, :, LAT:T] for p in parts], axis=2)
    return np.ascontiguousarray(np.concatenate([lat, ctx], axis=2))


def _values_layout(vt_all):
    C = vt_all.shape[0]
    v = vt_all.reshape(C, 128, NKB, 128)
    return np.ascontiguousarray(v.transpose(0, 3, 2, 1))


def _even_attention(layer, xT_shards, ml, mc, w_in, gq, gk, lam_v, subg):
    lambda_init = 0.8 - 0.6 * math.exp(-0.3 * layer)
    nca = _get("even_a", build_even_a)
    rotm = rot_matrix(128)
    sc, sh = fm2(ml[1], mc[1]), fm2(ml[0], mc[0])
    in_maps = []
    for j in range(NCORES):
        cos, sin = rope_tables(j, 128)
        in_maps.append({"xT": xT_shards[j], "w_in": w_in, "sc": sc, "sh": sh,
                        "cosT": cos, "sinT": sin, "gq": np.ascontiguousarray(gq.reshape(128, 1)),
                        "gk": np.ascontiguousarray(gk.reshape(128, 1)), "rotm": rotm})
    ra = run(nca, in_maps)
    kt_all = _gather_keys([r["KT"] for r in ra])
    v_all = _values_layout(_gather_keys([r["VT"] for r in ra]))
    ncb = _get("even_b1", build_even_b1, lambda_init)
    lamv = np.ascontiguousarray(lam_v.T)
    sg = np.ascontiguousarray(subg.reshape(2, 128).T)
    in_maps = [{"QT": ra[j]["QT"], "KTall": kt_all, "Vall": v_all, "lamv": lamv, "subg": sg}
               for j in range(NCORES)]
    rb = run(ncb, in_maps)
    return [r["attnT"] for r in rb]


def _odd_attention(xT_shards, ml, mc, w_in, gqn, gkvn, w_uq, w_ukv):
    nca = _get("odd_a", build_odd_a)
    rotm = rot_matrix(64)
    sc, sh = fm2(ml[1], mc[1]), fm2(ml[0], mc[0])
    gq = np.ascontiguousarray(gqn.reshape(8, 128).T)
    gk = np.ascontiguousarray(gkvn.reshape(4, 128).T)
    in_maps = []
    for j in range(NCORES):
        cos, sin = rope_tables(j, 64)
        in_maps.append({"xT": xT_shards[j], "w_in": w_in, "w_uq": w_uq, "w_ukv": w_ukv, "sc": sc, "sh": sh,
                        "cosT": cos, "sinT": sin, "gqn": gq, "gkvn": gk, "rotm": rotm})
    ra = run(nca, in_maps)
    kn_all = _gather_keys([r["KN"] for r in ra])
    v_all = _values_layout(_gather_keys([r["VT"] for r in ra]))
    kr_all = np.ascontiguousarray(_gather_keys([r["KR"][None] for r in ra])[0])
    ncb = _get("odd_b1", build_odd_b1)
    in_maps = [{"QN": ra[j]["QN"], "QR": ra[j]["QR"], "KNall": kn_all, "KRall": kr_all, "Vall": v_all}
               for j in range(NCORES)]
    rb = run(ncb, in_maps)
    return [r["attnT"] for r in rb]


def _post(attnT, xT_shards, ml, mc, w_out, ln1g, ln1b, ln2g, ln2b, w_group, b_group, w_router, b_router,
          w_gate, w_up, w_down):
    nc = _get("post", build_post)
    common = {
        "w_out": w_out, "w_gate": w_gate, "w_up": w_up,
        "w_down": np.ascontiguousarray(w_down.reshape(NEXP * DEXP, D)),
        "wgr": np.ascontiguousarray(np.concatenate([w_group, w_router], axis=1)),
        "bgr": np.ascontiguousarray(np.broadcast_to(np.concatenate([b_group, b_router])[None, :], (128, 20))),
        "g1": fm2(ml[2], mc[2]), "sc2": fm2(ml[4], mc[4]), "sh2": fm2(ml[3], mc[3]), "g2": fm2(ml[5], mc[5]),
        "ln1g": fm(ln1g), "ln1b": fm(ln1b), "ln2g": fm(ln2g), "ln2b": fm(ln2b),
        "identf": np.eye(128, dtype=np.float32), "selm": sel_matrix()}
    in_maps = [dict(common, attnT=attnT[j], xT=xT_shards[j]) for j in range(NCORES)]
    res = run(nc, in_maps)
    return [r["xT_new"] for r in res]


def kernel(x, c, ctx, c_ctx, w_ada, b_ada, ln1_g, ln1_b, ln2_g, ln2_b,
           ev_w_in, ev_w_out, diff_lambda, diff_subln_g, gqa_q_norm_g, gqa_k_norm_g,
           od_w_in, mla_q_norm_g, mla_kv_norm_g, mla_w_uq, mla_w_ukv, od_w_out,
           moe_w_group, moe_b_group, moe_w_router, moe_b_router, moe_w_gate, moe_w_up, moe_w_down):
    x = _f32(x)
    ctx = _f32(ctx)
    mods_l, mods_c = _mods_all(_f32(c), _f32(c_ctx), _f32(w_ada), _f32(b_ada))
    xT = [shard_tokens_T(x[0], ctx[0], j) for j in range(NCORES)]
    for layer in range(DEPTH):
        ml, mc = mods_l[layer], mods_c[layer]
        i = layer // 2
        if layer % 2 == 0:
            attnT = _even_attention(layer, xT, ml, mc, _f32(ev_w_in[i]), _f32(gqa_q_norm_g[i]),
                                    _f32(gqa_k_norm_g[i]), _f32(diff_lambda[i]), _f32(diff_subln_g[i]))
            w_out = _f32(ev_w_out[i])
        else:
            attnT = _odd_attention(xT, ml, mc, _f32(od_w_in[i]), _f32(mla_q_norm_g[i]), _f32(mla_kv_norm_g[i]),
                                   _f32(mla_w_uq[i]), _f32(mla_w_ukv[i]))
            w_out = _f32(od_w_out[i])
        xT = _post(attnT, xT, ml, mc, w_out, _f32(ln1_g[layer]), _f32(ln1_b[layer]), _f32(ln2_g[layer]),
                   _f32(ln2_b[layer]), _f32(moe_w_group[layer]), _f32(moe_b_group[layer]),
                   _f32(moe_w_router[layer]), _f32(moe_b_router[layer]), _f32(moe_w_gate[layer]),
                   _f32(moe_w_up[layer]), _f32(moe_w_down[layer]))
    out = np.concatenate([xT[j][:, 0:LAT].T for j in range(NCORES)], axis=0)
    return np.ascontiguousarray(out.reshape(1, SEQ, D).astype(np.float32))
```
